# Optimizing a Trainium2 kernel written in Bass

```python
import math
import jax, jax.numpy as jnp
from jax import lax
import numpy as np

D_MODEL = 1024
BATCH = 4
SEQ = 4096
DEPTH = 1
DEC_BATCH = 8
DEC_SEQ = 16
PAST_LEN = 1024

CHUNK = 64
QBLOCK = 128
ROPE_THETA = 10000.0
LN_EPS = 1e-5
GLA_HEADS = 4
GLA_DK = D_MODEL // 2 // GLA_HEADS
GLA_DV = D_MODEL // GLA_HEADS
GLA_GATE_RANK = 16
GLA_TAU = 16.0
DSA_HEADS = 8
DSA_KV_HEADS = 2
DSA_HEAD_DIM = D_MODEL // DSA_HEADS
IDX_HEADS = 8
IDX_DIM = 64
TOPK_MAX = 256
IDX_W_SCALE = (IDX_HEADS ** -0.5) * (IDX_DIM ** -0.5)
N_GROUPS = 4
EXPERTS_PER_GROUP = 4
N_EXPERTS = N_GROUPS * EXPERTS_PER_GROUP
D_EXPERT = 256
TOP_K_IN_GROUP = 2
ALPHA = (2 * DEPTH) ** 0.25
BETA = (8 * DEPTH) ** -0.25

_SPLIT_SIZES = (GLA_HEADS * GLA_DK, GLA_HEADS * GLA_DK, GLA_HEADS * GLA_DV, GLA_HEADS * GLA_DV, GLA_GATE_RANK,
                DSA_HEADS * DSA_HEAD_DIM, DSA_KV_HEADS * DSA_HEAD_DIM, DSA_KV_HEADS * DSA_HEAD_DIM,
                IDX_HEADS * IDX_DIM, IDX_DIM, IDX_HEADS, D_MODEL, D_MODEL)
D_IN_PROJ = sum(_SPLIT_SIZES)
_SPLIT_POINTS = tuple(sum(_SPLIT_SIZES[:i + 1]) for i in range(len(_SPLIT_SIZES) - 1))

kernel_name = 'hybrid_gla_dsa_hmoe_stream_step'


def layer_norm(x, g, b):
    xf = x.astype(jnp.float32)
    mu = jnp.mean(xf, axis=-1, keepdims=True)
    var = jnp.mean(jnp.square(xf - mu), axis=-1, keepdims=True)
    return ((xf - mu) * lax.rsqrt(var + LN_EPS) * g + b).astype(x.dtype)


def rope(x, pos):
    half = x.shape[-1] // 2
    inv = ROPE_THETA ** (-jnp.arange(half, dtype=jnp.float32) / half)
    ang = pos.astype(jnp.float32)[:, None] * inv[None, :]
    cos = jnp.cos(ang)[:, None, :]
    sin = jnp.sin(ang)[:, None, :]
    xf = x.astype(jnp.float32)
    x1, x2 = xf[..., :half], xf[..., half:]
    return jnp.concatenate([x1 * cos - x2 * sin, x2 * cos + x1 * sin], axis=-1).astype(x.dtype)


def gla_recurrence(q, k, v, logf, s0):
    B, L, H, DK = q.shape
    c = min(CHUNK, L)
    n = L // c

    def to_chunks(a):
        return a.reshape(B, n, c, *a.shape[2:]).swapaxes(0, 1)

    causal = jnp.tril(jnp.ones((c, c), dtype=bool))[None, :, :, None, None]

    def step(s, blk):
        qc, kc, vc, fc = blk
        b = jnp.cumsum(fc, axis=1)
        o_inter = jnp.einsum('bihk,bhkv->bihv', qc * jnp.exp(b), s)
        decay = jnp.exp(jnp.where(causal, b[:, :, None] - b[:, None, :], -jnp.inf))
        a = jnp.einsum('bihk,bjhk,bijhk->bhij', qc, kc, decay)
        o_intra = jnp.einsum('bhij,bjhv->bihv', a, vc)
        b_last = b[:, -1]
        k_dec = kc * jnp.exp(b_last[:, None] - b)
        s_new = jnp.exp(b_last)[..., None] * s + jnp.einsum('bjhk,bjhv->bhkv', k_dec, vc)
        return s_new, o_inter + o_intra

    s_fin, o = lax.scan(step, s0, (to_chunks(q), to_chunks(k), to_chunks(v), to_chunks(logf)))
    o = o.swapaxes(0, 1).reshape(B, L, H, v.shape[-1])
    return o, s_fin


def dsa_block(q, qi, wi, tpos, k_all, v_all, ki_all, kpos, topk):
    B, T = q.shape[0], q.shape[1]
    score = jnp.einsum('bthd,bsd->bths', qi, ki_all)
    idx_score = jnp.einsum('bth,bths->bts', wi, jax.nn.relu(score)).astype(jnp.float32)
    limit = (tpos // CHUNK + 1) * CHUNK
    admissible = kpos[None, :] < limit[:, None]
    idx_score = jnp.where(admissible[None], idx_score, -jnp.inf)
    sel_val, sel = lax.top_k(idx_score, topk)
    gather = jax.vmap(lambda a, i: a[i])
    kg = gather(k_all, sel)
    vg = gather(v_all, sel)
    qg = q.reshape(B, T, DSA_KV_HEADS, DSA_HEADS // DSA_KV_HEADS, DSA_HEAD_DIM)
    logits = jnp.einsum('btgrd,btkgd->btgrk', qg, kg).astype(jnp.float32) * (DSA_HEAD_DIM ** -0.5)
    logits = jnp.where(jnp.isfinite(sel_val)[:, :, None, None, :], logits, -jnp.inf)
    p = jax.nn.softmax(logits, axis=-1).astype(vg.dtype)
    o = jnp.einsum('btgrk,btkgd->btgrd', p, vg)
    return o.reshape(B, T, DSA_HEADS * DSA_HEAD_DIM)


def token_mixer(x, gla_s0, past_k, past_v, past_ki, w_in, w_gla_gate_up, b_gla_gate, gla_norm_g,
                gla_norm_b, w_branch_gla, w_branch_dsa, w_out):
    B, T, _ = x.shape
    past_len = past_k.shape[1]
    pos = past_len + jnp.arange(T, dtype=jnp.int32)
    z = x @ w_in
    (gq, gk, gv, gg, gr, dq, dk, dv, iq, ik, iw, ga, gb) = jnp.split(z, _SPLIT_POINTS, axis=-1)
    f32 = jnp.float32
    q = gq.reshape(B, T, GLA_HEADS, GLA_DK).astype(f32) * (GLA_DK ** -0.5)
    k = gk.reshape(B, T, GLA_HEADS, GLA_DK).astype(f32)
    v = gv.reshape(B, T, GLA_HEADS, GLA_DV).astype(f32)
    logf = jax.nn.log_sigmoid((gr @ w_gla_gate_up + b_gla_gate).astype(f32)) / GLA_TAU
    logf = logf.reshape(B, T, GLA_HEADS, GLA_DK)
    o, s_fin = gla_recurrence(q, k, v, logf, gla_s0.astype(f32))
    o = layer_norm(o, gla_norm_g, gla_norm_b).astype(x.dtype) * jax.nn.silu(gg).reshape(B, T, GLA_HEADS, GLA_DV)
    o_gla = o.reshape(B, T, GLA_HEADS * GLA_DV)
    qd = rope(dq.reshape(B, T, DSA_HEADS, DSA_HEAD_DIM), pos)
    k_new = rope(dk.reshape(B, T, DSA_KV_HEADS, DSA_HEAD_DIM), pos)
    v_new = dv.reshape(B, T, DSA_KV_HEADS, DSA_HEAD_DIM)
    qi = rope(iq.reshape(B, T, IDX_HEADS, IDX_DIM), pos)
    ki_new = rope(ik.reshape(B, T, 1, IDX_DIM), pos)[:, :, 0]
    wi = iw * IDX_W_SCALE
    k_all = jnp.concatenate([past_k.astype(x.dtype), k_new], axis=1)
    v_all = jnp.concatenate([past_v.astype(x.dtype), v_new], axis=1)
    ki_all = jnp.concatenate([past_ki.astype(x.dtype), ki_new], axis=1)
    L = past_len + T
    kpos = jnp.arange(L, dtype=jnp.int32)
    topk = min(TOPK_MAX, L // 4)
    qb = min(QBLOCK, T)
    nb = T // qb

    def to_blocks(a):
        return a.reshape(B, nb, qb, *a.shape[2:]).swapaxes(0, 1)

    def attend(blk):
        bq, bqi, bwi, btp = blk
        return dsa_block(bq, bqi, bwi, btp, k_all, v_all, ki_all, kpos, topk)

    o_dsa = lax.map(attend, (to_blocks(qd), to_blocks(qi), to_blocks(wi), pos.reshape(nb, qb)))
    o_dsa = o_dsa.swapaxes(0, 1).reshape(B, T, DSA_HEADS * DSA_HEAD_DIM)
    m = jax.nn.sigmoid(ga) * (o_gla @ w_branch_gla) + jax.nn.sigmoid(gb) * (o_dsa @ w_branch_dsa)
    return m @ w_out, s_fin, k_new, v_new, ki_new


def hier_moe(h, w_router_group, b_router_group, w_router_expert, b_router_expert, w_expert_gate,
             w_expert_up, w_expert_down):
    B, T, D = h.shape
    xt = h.reshape(B * T, D)
    g_logits = (xt @ w_router_group + b_router_group).astype(jnp.float32)
    g_prob = jax.nn.softmax(g_logits, axis=-1)
    g_sel = jnp.argmax(g_logits, axis=-1)
    e_logits = (jnp.einsum('nd,gde->nge', xt, w_router_expert) + b_router_expert).astype(jnp.float32)
    e_logits = jnp.take_along_axis(e_logits, g_sel[:, None, None], axis=1)[:, 0]
    top_val, top_idx = lax.top_k(e_logits, TOP_K_IN_GROUP)
    top_w = jax.nn.softmax(top_val, axis=-1) * jnp.take_along_axis(g_prob, g_sel[:, None], axis=1)
    expert_id = g_sel[:, None] * EXPERTS_PER_GROUP + top_idx
    gate = jnp.sum(jax.nn.one_hot(expert_id, N_EXPERTS, dtype=jnp.float32) * top_w[..., None], axis=1)
    hid = jax.nn.silu(jnp.einsum('nd,edf->nef', xt, w_expert_gate)) * jnp.einsum('nd,edf->nef', xt, w_expert_up)
    hid = (hid * gate[:, :, None]).astype(h.dtype)
    out = jnp.einsum('nef,efd->nd', hid, w_expert_down)
    return out.reshape(B, T, D).astype(h.dtype)


def trunk_layer(x, gla_s0, past_k, past_v, past_ki, w_in, w_gla_gate_up, b_gla_gate, gla_norm_g, gla_norm_b,
                w_branch_gla, w_branch_dsa, w_out, ln1_g, ln1_b, w_router_group, b_router_group,
                w_router_expert, b_router_expert, w_expert_gate, w_expert_up, w_expert_down, ln2_g, ln2_b):
    mix, s_fin, k_new, v_new, ki_new = token_mixer(x, gla_s0, past_k, past_v, past_ki, w_in, w_gla_gate_up,
                                                   b_gla_gate, gla_norm_g, gla_norm_b, w_branch_gla,
                                                   w_branch_dsa, w_out)
    h = layer_norm(ALPHA * x + mix, ln1_g, ln1_b)
    ffn = hier_moe(h, w_router_group, b_router_group, w_router_expert, b_router_expert, w_expert_gate,
                   w_expert_up, w_expert_down)
    y = layer_norm(ALPHA * h + ffn, ln2_g, ln2_b)
    return y, s_fin, k_new, v_new, ki_new


def setup_inputs(seed: int = 0) -> dict:
    key = jax.random.key(seed)
    ks = jax.random.split(key, 32)
    f32 = jnp.float32
    nrm = lambda k, shape, s: jax.random.normal(k, shape, f32) * s
    return {
        'x_prompt': nrm(ks[0], (BATCH, SEQ, D_MODEL), 1.0),
        'x_sample': nrm(ks[1], (DEC_BATCH, DEC_SEQ, D_MODEL), 1.0),
        'state_gla': nrm(ks[2], (DEPTH, DEC_BATCH, GLA_HEADS, GLA_DK, GLA_DV), 0.5),
        'cache_k': nrm(ks[3], (DEPTH, DEC_BATCH, PAST_LEN, DSA_KV_HEADS, DSA_HEAD_DIM), 1.0),
        'cache_v': nrm(ks[4], (DEPTH, DEC_BATCH, PAST_LEN, DSA_KV_HEADS, DSA_HEAD_DIM), 1.0),
        'cache_k_idx': nrm(ks[5], (DEPTH, DEC_BATCH, PAST_LEN, IDX_DIM), 1.0),
        'w_in': nrm(ks[6], (DEPTH, D_MODEL, D_IN_PROJ), D_MODEL ** -0.5),
        'w_gla_gate_up': nrm(ks[7], (DEPTH, GLA_GATE_RANK, GLA_HEADS * GLA_DK), GLA_GATE_RANK ** -0.5),
        'b_gla_gate': nrm(ks[8], (DEPTH, GLA_HEADS * GLA_DK), 0.1),
        'gla_norm_g': 1.0 + nrm(ks[9], (DEPTH, GLA_DV), 0.02),
        'gla_norm_b': nrm(ks[10], (DEPTH, GLA_DV), 0.02),
        'w_branch_gla': nrm(ks[11], (DEPTH, GLA_HEADS * GLA_DV, D_MODEL), (GLA_HEADS * GLA_DV) ** -0.5),
        'w_branch_dsa': nrm(ks[12], (DEPTH, DSA_HEADS * DSA_HEAD_DIM, D_MODEL), (DSA_HEADS * DSA_HEAD_DIM) ** -0.5),
        'w_out': nrm(ks[13], (DEPTH, D_MODEL, D_MODEL), BETA * D_MODEL ** -0.5),
        'ln1_g': 1.0 + nrm(ks[14], (DEPTH, D_MODEL), 0.02),
        'ln1_b': nrm(ks[15], (DEPTH, D_MODEL), 0.02),
        'w_router_group': nrm(ks[16], (DEPTH, D_MODEL, N_GROUPS), D_MODEL ** -0.5),
        'b_router_group': nrm(ks[17], (DEPTH, N_GROUPS), 0.01),
        'w_router_expert': nrm(ks[18], (DEPTH, N_GROUPS, D_MODEL, EXPERTS_PER_GROUP), D_MODEL ** -0.5),
        'b_router_expert': nrm(ks[19], (DEPTH, N_GROUPS, EXPERTS_PER_GROUP), 0.01),
        'w_expert_gate': nrm(ks[20], (DEPTH, N_EXPERTS, D_MODEL, D_EXPERT), D_MODEL ** -0.5),
        'w_expert_up': nrm(ks[21], (DEPTH, N_EXPERTS, D_MODEL, D_EXPERT), D_MODEL ** -0.5),
        'w_expert_down': nrm(ks[22], (DEPTH, N_EXPERTS, D_EXPERT, D_MODEL), BETA * D_EXPERT ** -0.5),
        'ln2_g': 1.0 + nrm(ks[23], (DEPTH, D_MODEL), 0.02),
        'ln2_b': nrm(ks[24], (DEPTH, D_MODEL), 0.02),
    }


def reference(x_prompt, x_sample, state_gla, cache_k, cache_v, cache_k_idx, w_in, w_gla_gate_up, b_gla_gate,
              gla_norm_g, gla_norm_b, w_branch_gla, w_branch_dsa, w_out, ln1_g, ln1_b, w_router_group,
              b_router_group, w_router_expert, b_router_expert, w_expert_gate, w_expert_up, w_expert_down,
              ln2_g, ln2_b):
    hp, hs = x_prompt, x_sample
    B = x_prompt.shape[0]
    gla_p, k_p, v_p, ki_p = [], [], [], []
    gla_s, k_s, v_s, ki_s = [], [], [], []
    for l in range(DEPTH):
        lw = (w_in[l], w_gla_gate_up[l], b_gla_gate[l], gla_norm_g[l], gla_norm_b[l], w_branch_gla[l],
              w_branch_dsa[l], w_out[l], ln1_g[l], ln1_b[l], w_router_group[l], b_router_group[l],
              w_router_expert[l], b_router_expert[l], w_expert_gate[l], w_expert_up[l], w_expert_down[l],
              ln2_g[l], ln2_b[l])
        s0 = jnp.zeros((B, GLA_HEADS, GLA_DK, GLA_DV), jnp.float32)
        ek = jnp.zeros((B, 0, DSA_KV_HEADS, DSA_HEAD_DIM), x_prompt.dtype)
        eki = jnp.zeros((B, 0, IDX_DIM), x_prompt.dtype)
        hp, sp, kp, vp, kip = trunk_layer(hp, s0, ek, ek, eki, *lw)
        hs, ss, ksn, vsn, kisn = trunk_layer(hs, state_gla[l], cache_k[l], cache_v[l], cache_k_idx[l], *lw)
        gla_p.append(sp); k_p.append(kp); v_p.append(vp); ki_p.append(kip)
        gla_s.append(ss); k_s.append(ksn); v_s.append(vsn); ki_s.append(kisn)
    return (hp, hs, jnp.stack(gla_p), jnp.stack(k_p), jnp.stack(v_p), jnp.stack(ki_p),
            jnp.stack(gla_s), jnp.stack(k_s), jnp.stack(v_s), jnp.stack(ki_s))
```

```python
import contextlib
import numpy as np
import concourse.bass as bass
import concourse.mybir as mybir
from concourse.bass_utils import run_bass_kernel_spmd

F32 = mybir.dt.float32
BF16 = mybir.dt.bfloat16
AF = mybir.ActivationFunctionType
ALU = mybir.AluOpType
AX = mybir.AxisListType

D = 1024
SEQ = 4096
NPAIR = 16
DEC_T = 16
PAST = 1024
DK = 128
DV = 256
NIT = 16
TOPK = 256
NEG = -30000.0
BSCALE = 0.75
ALPHA = 2.0 ** 0.25
IDX_W_SCALE = (8 ** -0.5) * (64 ** -0.5)
LN_EPS = 1e-5
ATT_SCALE = 128 ** -0.5

_SZ = (512, 512, 1024, 1024, 16, 1024, 256, 256, 512, 64, 8, 1024, 1024)
_NM = ("gq", "gk", "gv", "gg", "gr", "dq", "dk", "dv", "iq", "ik", "iw", "ga", "gb")
_OFF = {}
_o = 0
for _n, _s in zip(_NM, _SZ):
    _OFF[_n] = (_o, _o + _s)
    _o += _s

_BLK = [
    ("sm", [("in", *_OFF["gr"]), ("in", *_OFF["ik"]), ("in", *_OFF["iw"])]),
    ("gk", [("in", *_OFF["gk"])]),
    ("gv0", [("in", 1024, 1536)]),
    ("gv1", [("in", 1536, 2048)]),
    ("dkv", [("in", *_OFF["dk"]), ("in", *_OFF["dv"])]),
    ("gq", [("in", *_OFF["gq"])]),
    ("gg0", [("in", 2048, 2560)]),
    ("gg1", [("in", 2560, 3072)]),
    ("dq0", [("in", 3088, 3600)]),
    ("dq1", [("in", 3600, 4112)]),
    ("iq", [("in", *_OFF["iq"])]),
    ("ga0", [("in", 5208, 5720)]),
    ("wg0", [("bg", 0, 512)]),
    ("gb0", [("in", 6232, 6744)]),
    ("wd0", [("bd", 0, 512)]),
    ("ga1", [("in", 5720, 6232)]),
    ("wg1", [("bg", 512, 1024)]),
    ("gb1", [("in", 6744, 7256)]),
    ("wd1", [("bd", 512, 1024)]),
    ("wo0", [("wo", 0, 512)]),
    ("wo1", [("wo", 512, 1024)]),
]
_BLK_COLS = {}
_BLK_OFF = {}
_t = 0
for _n, _parts in _BLK:
    _c = sum(b - a for _, a, b in _parts)
    _BLK_COLS[_n] = _c
    _BLK_OFF[_n] = _t
    _t += 8 * _c
WA_TOT = _t
_BLK_ORDER = [n for n, _ in _BLK]

WE_PER = 8 * 512 + 2 * 1024
WE_TOT = 16 * WE_PER

_C = {}
_t = 0
for _n, _s in (("ident", 128), ("uneg", 128), ("cm", 128), ("pow2", 32), ("pm", 2), ("admb", 256)):
    _C[_n] = (_t, _t + _s)
    _t += _s
C_TOT = _t


class Prog:
    ENGS = ("pe", "act", "dve", "pool", "sp")

    def __init__(self, n_dma=24, n_fresh=40):
        self.n_fresh = n_fresh
        self.fresh_used = 0
        self.ops = {e: [] for e in self.ENGS}
        self.cnt = {e: 0 for e in self.ENGS}
        self.known = {e: {} for e in self.ENGS}
        self.last_w = {}
        self.readers = {}
        self.n_dma = n_dma
        self.dma_cnt = [0] * (n_dma + n_fresh)
        self.rr = 0

    def _collect(self, eng, reads, writes):
        deps = {}

        def add(sk, v):
            if deps.get(sk, 0) < v:
                deps[sk] = v

        for k in reads:
            if k in self.last_w:
                add(*self.last_w[k])
        for k in writes:
            if k in self.last_w:
                add(*self.last_w[k])
            for sk, v in self.readers.get(k, {}).items():
                add(sk, v)
        waits = []
        kn = self.known[eng]
        for sk, v in deps.items():
            if sk == "pe" and eng == "pe":
                continue
            if kn.get(sk, 0) >= v:
                continue
            kn[sk] = v
            waits.append((sk, v))
        return waits

    def _register(self, ev, reads, writes):
        sk, v = ev
        for k in reads:
            r = self.readers.setdefault(k, {})
            if r.get(sk, 0) < v:
                r[sk] = v
        for k in writes:
            self.last_w[k] = ev
            self.readers[k] = {}

    def op(self, eng, fn, reads=(), writes=()):
        waits = self._collect(eng, reads, writes)
        self.cnt[eng] += 1
        ev = (eng, self.cnt[eng])
        self._register(ev, reads, writes)
        self.ops[eng].append((waits, fn, eng))

    def dma(self, q, fn, reads=(), writes=(), fresh=False):
        if fresh:
            k = self.n_dma + self.fresh_used
            self.fresh_used += 1
            assert self.fresh_used <= self.n_fresh
        else:
            k = self.rr % self.n_dma
            self.rr += 1
        waits = self._collect(q, reads, writes)
        sk = ("dma", k)
        if self.dma_cnt[k] > 0 and self.known[q].get(sk, 0) < self.dma_cnt[k]:
            self.known[q][sk] = self.dma_cnt[k]
            waits.append((sk, self.dma_cnt[k]))
        self.dma_cnt[k] += 1
        ev = (sk, self.dma_cnt[k])
        self._register(ev, reads, writes)
        self.ops[q].append((waits, fn, sk))

    def barrier(self):
        snap = dict(self.cnt)
        dsnap = list(self.dma_cnt)
        for e in self.ENGS:
            kn = self.known[e]
            waits = []
            for sk, v in snap.items():
                if v > 0 and sk != e and kn.get(sk, 0) < v:
                    kn[sk] = v
                    waits.append((sk, v))
            for k, v in enumerate(dsnap):
                sk = ("dma", k)
                if v > 0 and kn.get(sk, 0) < v:
                    kn[sk] = v
                    waits.append((sk, v))
            if waits:
                self.ops[e].append((waits, None, None))
        self.last_w = {}
        self.readers = {}


class MK(tuple):
    pass


def _keys(k):
    if k is None:
        return []
    if isinstance(k, MK):
        return list(k)
    return [k]


class Buf:
    __slots__ = ("ap", "key", "ps")

    def __init__(self, ap, key, ps=False):
        self.ap = ap
        self.key = key
        self.ps = ps

    def __getitem__(self, idx):
        return Buf(self.ap[idx], self.key, self.ps)

    def rr(self, s, **kw):
        return Buf(self.ap.rearrange(s, **kw), self.key, self.ps)

    def bc(self, dt):
        return Buf(self.ap.bitcast(dt), self.key, self.ps)

    def uq(self, ax):
        return Buf(self.ap.unsqueeze(ax), self.key, self.ps)

    def bt(self, shape):
        return Buf(self.ap.broadcast_to(list(shape)), self.key, self.ps)

    def rk(self, key):
        return Buf(self.ap, key, self.ps)


class Arena:
    def __init__(self, big, nbytes):
        self.big = big
        self.off = 0
        self.cap = nbytes
        self.n = 0

    def alloc(self, free_shape, dtype, parts=128, key=None):
        esz = 2 if dtype == BF16 else 4
        n = int(np.prod(free_shape))
        nb = (n * esz + 31) // 32 * 32
        assert self.off + nb <= self.cap, f"SBUF arena overflow {self.off + nb} > {self.cap}"
        ap = self.big[0:parts, self.off // 4:(self.off + nb) // 4]
        if dtype == BF16:
            ap = ap.bitcast(BF16)
        ap = ap[:, 0:n]
        if len(free_shape) == 2:
            ap = ap.rearrange("p (a b) -> p a b", a=free_shape[0])
        elif len(free_shape) == 3:
            ap = ap.rearrange("p (a b c) -> p a b c", a=free_shape[0], b=free_shape[1])
        self.off += nb
        self.n += 1
        return Buf(ap, key if key is not None else ("sb", self.n))


class K:
    def __init__(self, stage, wseq=None):
        self.stage = stage
        self.prog = Prog()
        self.nc = bass.Bass("TRN2", target_bir_lowering=False)
        self.plan = wseq is None
        self.wseq = wseq if wseq is not None else []

    def E(self, eng, fn, outs, ins):
        reads = [k for b in ins if not b.ps for k in _keys(b.key)]
        writes = [k for b in outs for k in _keys(b.key)] + [k for b in ins if b.ps for k in _keys(b.key)]
        self.prog.op(eng, fn, reads, writes)

    def raw(self, eng, name, outs, ins, **kw):
        kw2 = {k: (v.ap if isinstance(v, Buf) else v) for k, v in kw.items()}
        self.E(eng, lambda e: getattr(e, name)(**kw2), outs, ins)

    def mm(self, out, lhsT, rhs, start=True, stop=True, skip=False):
        o, l, r = out.ap, lhsT.ap, rhs.ap
        self.E("pe", lambda e: e.matmul(o, lhsT=l, rhs=r, start=start, stop=stop, skip_group_check=skip),
               [out], [lhsT, rhs])

    def tr(self, out, in_, ident):
        o, i, d = out.ap, in_.ap, ident.ap
        self.E("pe", lambda e: e.transpose(o, i, d), [out], [in_, ident])

    def act(self, out, in_, func, bias=None, scale=None, accum=None):
        o, i = out.ap, in_.ap
        kw = {}
        ins = [in_]
        outs = [out]
        if bias is not None:
            if isinstance(bias, Buf):
                kw["bias"] = bias.ap
                ins.append(bias)
            else:
                kw["bias"] = float(bias)
        if scale is not None:
            if isinstance(scale, Buf):
                kw["scale"] = scale.ap
                ins.append(scale)
            else:
                kw["scale"] = float(scale)
        if accum is not None:
            kw["accum_out"] = accum.ap
            outs.append(accum)
        self.E("act", lambda e: e.activation(out=o, in_=i, func=func, **kw), outs, ins)

    def tt(self, out, in0, in1, op, eng="dve"):
        o, a, b = out.ap, in0.ap, in1.ap
        self.E(eng, lambda e: e.tensor_tensor(out=o, in0=a, in1=b, op=op), [out], [in0, in1])

    def ts(self, out, in0, s1, op0, s2=None, op1=None, accum=None, eng="dve"):
        o, a = out.ap, in0.ap
        ins = [in0]
        outs = [out]
        if isinstance(s1, Buf):
            ins.append(s1)
            s1v = s1.ap
        else:
            s1v = float(s1)
        if isinstance(s2, Buf):
            ins.append(s2)
            s2v = s2.ap
        else:
            s2v = None if s2 is None else float(s2)
        kw = {}
        if op1 is not None:
            kw["op1"] = op1
        if accum is not None:
            kw["accum_out"] = accum.ap
            outs.append(accum)
        self.E(eng, lambda e: e.tensor_scalar(out=o, in0=a, scalar1=s1v, scalar2=s2v, op0=op0, **kw), outs, ins)

    def stt(self, out, in0, scalar, in1, op0, op1):
        o, a, b = out.ap, in0.ap, in1.ap
        ins = [in0, in1]
        if isinstance(scalar, Buf):
            ins.append(scalar)
            sv = scalar.ap
        else:
            sv = float(scalar)
        self.E("dve", lambda e: e.scalar_tensor_tensor(out=o, in0=a, scalar=sv, in1=b, op0=op0, op1=op1), [out], ins)

    def cp(self, out, in_, eng="dve"):
        o, i = out.ap, in_.ap
        if eng == "act":
            self.E("act", lambda e: e.activation(out=o, in_=i, func=AF.Copy), [out], [in_])
        else:
            self.E(eng, lambda e: e.tensor_copy(o, i), [out], [in_])

    def memset(self, out, val, eng="dve"):
        o = out.ap
        self.E(eng, lambda e: e.memset(o, float(val)), [out], [])

    def dma(self, q, out, in_, fresh=None):
        o, i = out.ap, in_.ap
        if fresh is None:
            fresh = (q == "pool")
        self.prog.dma(q, lambda e: e.dma_start(out=o, in_=i), _keys(in_.key), _keys(out.key), fresh=fresh)

    def build(self):
        nc = self.nc
        dr = lambda name, shape, kind="ExternalInput": Buf(nc.dram_tensor(name, list(shape), F32, kind=kind).ap(), None)
        I = self.inp = {}
        I["x_seq"] = dr("x_seq", [SEQ, D])
        I["x_own"] = dr("x_own", [SEQ // 2, D])
        I["x_smp"] = dr("x_smp", [DEC_T, D])
        I["s0"] = dr("s0", [4, DK, DV])
        I["ck"] = dr("ck", [PAST, 256])
        I["cv"] = dr("cv", [PAST, 256])
        I["cki"] = dr("cki", [PAST, 64])
        I["wa"] = dr("wa", [128, WA_TOT])
        I["we"] = dr("we", [128, WE_TOT])
        I["wup"] = dr("wup", [17, 512])
        I["gn"] = dr("gn", [2, 256])
        I["ln"] = dr("ln", [4, D])
        I["wr"] = dr("wr", [128, 8 * 20])
        I["br"] = dr("br", [1, 20])
        I["sel"] = dr("sel", [16, 16 * 128])
        I["cst"] = dr("cst", [128, C_TOT])
        I["rope_seq"] = dr("rope_seq", [SEQ, 192])
        I["rope_own"] = dr("rope_own", [SEQ // 2, 192])
        O = self.out = {}
        O["y_own"] = dr("y_own", [SEQ // 2, D], "ExternalOutput")
        O["y_smp"] = dr("y_smp", [DEC_T, D], "ExternalOutput")
        O["st_p"] = dr("st_p", [4, DK, DV], "ExternalOutput")
        O["k_p"] = dr("k_p", [SEQ, 256], "ExternalOutput")
        O["v_p"] = dr("v_p", [SEQ, 256], "ExternalOutput")
        O["ki_p"] = dr("ki_p", [SEQ, 64], "ExternalOutput")
        O["st_s"] = dr("st_s", [4, DK, DV], "ExternalOutput")
        O["k_s"] = dr("k_s", [DEC_T, 256], "ExternalOutput")
        O["v_s"] = dr("v_s", [DEC_T, 256], "ExternalOutput")
        O["ki_s"] = dr("ki_s", [DEC_T, 64], "ExternalOutput")
        self.hscr = Buf(nc.dram_tensor("hscr", [17 * 128, D], F32, kind="Internal").ap(), "hscr")
        self.wa16 = Buf(nc.dram_tensor("wa16", [128, WA_TOT], BF16, kind="Internal").ap(), "wa16")
        self.we16 = Buf(nc.dram_tensor("we16", [128, WE_TOT], BF16, kind="Internal").ap(), "we16")

        SB_BYTES = 212832
        with contextlib.ExitStack() as es:
            big = es.enter_context(nc.sbuf_tensor("big", [128, SB_BYTES // 4], F32))
            self.ar = Arena(big, SB_BYTES)
            self.psb = []
            for k in range(8):
                t = es.enter_context(nc.psum_tensor(f"ps{k}", [128, 512], F32))
                self.psb.append(Buf(t[:], ("ps", k), True))
            sems = {e: es.enter_context(nc.semaphore(f"s_{e}")) for e in Prog.ENGS}
            dsems = [es.enter_context(nc.semaphore(f"d_{k}")) for k in range(self.prog.n_dma + self.prog.n_fresh)]

            self.program()
            if self.plan:
                return None

            block = es.enter_context(nc.Block())
            prog = self.prog

            def make(en):
                def body(e):
                    for waits, fn, inc in prog.ops[en]:
                        for sk, v in waits:
                            if isinstance(sk, tuple):
                                e.wait_ge(dsems[sk[1]], 16 * v)
                            else:
                                e.wait_ge(sems[sk], v)
                        if fn is None:
                            continue
                        ins = fn(e)
                        if isinstance(inc, tuple):
                            ins.then_inc(dsems[inc[1]], 16)
                        else:
                            ins.then_inc(sems[inc], 1)
                    if en == "sp":
                        for k in range(len(prog.dma_cnt)):
                            if prog.dma_cnt[k]:
                                e.wait_ge(dsems[k], 16 * prog.dma_cnt[k])
                        for sk in ("pe", "act", "dve", "pool"):
                            if prog.cnt[sk]:
                                e.wait_ge(sems[sk], prog.cnt[sk])
                return body

            block.tensor(make("pe"))
            block.scalar(make("act"))
            block.vector(make("dve"))
            block.gpsimd(make("pool"))
            block.sync(make("sp"))
        return nc

    def ps(self, k, dtype=F32):
        b = self.psb[k]
        return b.bc(BF16) if dtype == BF16 else b

    def program(self):
        ar = self.ar
        I, O = self.inp, self.out
        A = ar.alloc
        CH = 8192
        for c in range((WA_TOT + CH - 1) // CH):
            a, b = c * CH, min(WA_TOT, (c + 1) * CH)
            self.dma("pool", self.wa16[:, a:b].rk(("wa16", c)), I["wa"][:, a:b].rk(("wa16", c - 1) if c else None))
        self.CH = CH
        self.we_chunks = list(range((WE_TOT + CH - 1) // CH)) if self.stage >= 4 else []
        cst = A([C_TOT], F32)
        self.dma("sp", cst, I["cst"])
        cs = lambda n: cst[:, _C[n][0]:_C[n][1]]
        self.identf = cs("ident")
        self.uneg = cs("uneg")
        self.cm = cs("cm")
        self.pow2 = cs("pow2")
        self.pm = cs("pm")
        self.admb = cs("admb")
        self.ident = A([128], BF16)
        self.cp(self.ident, self.identf, "dve")
        self.i4 = A([4, 128], BF16)
        for h in range(4):
            self.cp(self.i4[:, h, :], self.identf, "dve")
        self.wup = A([512], F32, parts=17)
        self.dma("sp", self.wup, I["wup"])
        self.gn = A([2, 256], F32)
        self.dma("sp", self.gn[:, 0, :], Buf(I["gn"].ap[0:1, :].partition_broadcast(128), None))
        self.dma("sp", self.gn[:, 1, :], Buf(I["gn"].ap[1:2, :].partition_broadcast(128), None))
        self.grx = A([128], F32, parts=17)
        self.memset(self.grx, 1.0)
        self.eps = A([1], F32)
        self.memset(self.eps, LN_EPS)
        self.cbt = A([32], F32)

        self.p2_mark = ar.off
        NKT = 32
        self.KT = A([2, NKT * 128], BF16, key="KTall")
        self.VX = A([NKT, 2, 130], BF16, key="VXall")
        self.KI = A([NKT * 128], BF16, key="KIall")
        self.memset(self.VX[:, :, :, 128:130].rk("VXones"), 1.0, "pool")
        self.kvp = {"KT": self.KT, "VX": self.VX, "KI": self.KI, "n": ""}
        self.S = A([4, 256], F32, key="S")
        self.memset(self.S, 0.0)
        self.wring = [A([8 * 512], BF16, key=("wring", i)) for i in range(4)]
        self.wcons = 0
        self.wissued = 0

        sl = []
        for s in range(2):
            d = {}
            if s == 1:
                self.sl1_off = ar.off
            d["xT"] = A([8, 128], BF16)
            d["En"] = A([512], F32)
            d["EpT"] = A([4, 128], F32)
            d["Ktm"] = A([512], BF16)
            d["KtT"] = A([4, 128], BF16)
            d["Vg"] = A([1024], BF16)
            d["Sbf"] = A([4, 256], BF16)
            sl.append(d)
        self.t12 = A([1024], F32)
        self.t1 = self.t12[:, 0:512]
        self.t2 = self.t12[:, 512:1024]
        for d in sl:
            d["L"] = self.t1
            d["e1"] = self.t2
        self.sl = sl
        off1 = self.sl1_off
        reg = self.ar.big[:, off1 // 4:(off1 + 12288) // 4].bitcast(BF16)
        k1 = MK(d_.key for d_ in (sl[1][n_] for n_ in ("xT", "En", "EpT", "Ktm", "KtT", "Vg", "Sbf")))
        self.kvs = {"KT": Buf(reg[:, 0:2304].rearrange("p (g t) -> p g t", g=2), k1),
                    "VX": Buf(reg[:, 2304:2304 + 2340].rearrange("p (t g d) -> p t g d", t=9, g=2), k1),
                    "KI": Buf(reg[:, 4672:4672 + 1152], k1), "n": "s"}
        self.xs = [A([1024], F32) for _ in range(2)]
        self.xb = A([1024], BF16)
        self.rt = [A([192], F32) for _ in range(2)]
        self.knew = A([256], F32)
        self.vnew = A([256], F32)
        self.kinew = A([64], F32)
        self.kb16 = A([256], BF16)
        self.kidup = A([128], BF16)
        self.xrr = 0
        if self.stage >= 2:
            self.xo2 = [A([1024], F32)] * 3
            self.xres = A([1024], F32)
            self.xoT2 = [A([8, 128], BF16) for _ in range(3)]
            self.sab = A([512], BF16)
            self.tbb = A([512], BF16)
            self.rto2 = [A([192], F32) for _ in range(2)]
            self.wi2 = [A([8], F32) for _ in range(2)]
            self.KtT_o = A([4, 128], BF16)
            self.Vg_o = A([1024], BF16)
            self.EpT_o = A([4, 128], F32)
            self.Sbf_o = A([4, 256], BF16)
            self.QtT = A([4, 128], BF16)
            self.AT = A([4, 128], BF16)
            self.sgg = A([1024], BF16)
            self.bufA = self.t12
            self.stg = [A([1024], BF16) for _ in range(3)]
            self.ogT2 = [A([8, 128], BF16) for _ in range(2)]
            self.qT2 = [A([8, 128], BF16) for _ in range(2)]
            self.qir = A([512], BF16)
            self.qiT = A([4, 128], BF16)
            self.odT = A([8, 128], BF16)
            self.mT = A([8, 128], BF16)
            self.idx = A([4096], F32)
            self.mb = A([4096], BF16)
            self.rbuf = [A([512], F32) for _ in range(2)]
            self.PTb = [A([4, 128], BF16) for _ in range(3)]
            self.sm = A([128], F32)
            self.cstage = self.mb[:, 0:2048].rr("p (t c) -> p t c", t=8)
            self.kistage = self.mb[:, 2048:3072].rr("p (t c) -> p t c", t=8)
        print("phase-1 SBUF bytes/partition:", ar.off)

        self.prompt()
        if self.stage < 3:
            self.dma("sp", O["st_p"].rr("h k v -> k h v"), self.S)
        if self.stage >= 4:
            self.phase2()

    def load_xT(self, src_rows, nt, xT, xs=None, trb=2, load=True):
        if xs is None:
            xs = self.xs[self.xrr % 2]
            self.xrr += 1
        if load:
            self.dma("sp", xs[0:nt, :], src_rows)
        self.cp(self.xb[0:nt, :], xs[0:nt, :], "dve")
        pt = self.ps(trb, BF16)
        for k in range(8):
            self.tr(pt[:, k * nt:(k + 1) * nt], self.xb[0:nt, k * 128:(k + 1) * 128], self.ident[0:nt, 0:nt])
        self.cp(xT[:, :, 0:nt], pt[:, 0:8 * nt].rr("p (k t) -> p k t", k=8), "dve")
        return xs

    def rope(self, out, src, tab, nt, H, h):
        cosb, sinb = tab
        x4 = src.rr("p (a b c) -> p a b c", a=H, b=2)
        t1 = self.t1[0:nt, 0:H * 2 * h].rr("p (a b c) -> p a b c", a=H, b=2)
        t2 = self.t2[0:nt, 0:H * 2 * h].rr("p (a b c) -> p a b c", a=H, b=2)
        o4 = out.rr("p (a b c) -> p a b c", a=H, b=2)
        c4 = cosb.uq(1).uq(1).bt([nt, H, 2, h])
        s3 = sinb.uq(1).bt([nt, H, h])
        self.tt(t1, x4, c4, ALU.mult)
        self.tt(t2[:, :, 0, :], x4[:, :, 1, :], s3, ALU.mult)
        self.tt(t2[:, :, 1, :], x4[:, :, 0, :], s3, ALU.mult)
        self.tt(o4[:, :, 0, :], t1[:, :, 0, :], t2[:, :, 0, :], ALU.subtract)
        self.tt(o4[:, :, 1, :], t1[:, :, 1, :], t2[:, :, 1, :], ALU.add)

    def wget(self, name):
        if self.plan:
            self.wseq.append(name)
        assert self.wseq[self.wcons] == name, (self.wseq[self.wcons], name)
        while self.wissued < min(len(self.wseq), self.wcons + len(self.wring)):
            n = self.wseq[self.wissued]
            slot = self.wring[self.wissued % len(self.wring)]
            c = _BLK_COLS[n]
            off = _BLK_OFF[n]
            chs = MK(("wa16", q) for q in range(off // self.CH, (off + 8 * c - 1) // self.CH + 1))
            self.dma("sp", slot[:, 0:8 * c], self.wa16[:, off:off + 8 * c].rk(chs))
            self.wissued += 1
        slot = self.wring[self.wcons % len(self.wring)]
        c = _BLK_COLS[name]
        self.wcons += 1
        return slot[:, 0:8 * c].rr("p (k c) -> p k c", k=8)

    def sh_sm(self, s, nt, w, kt, ki_out, kv=None):
        kv = kv or self.kvp
        d = self.sl[s]
        xT = d["xT"]
        rt = self.rt[s]
        pg = self.ps(4)
        pt = self.ps(3, BF16)
        for k in range(8):
            self.mm(pg[0:16, 0:nt], w[:, k, 0:16], xT[:, k, 0:nt], start=(k == 0), stop=(k == 7))
        self.cp(self.grx[0:16, 0:nt], pg[0:16, 0:nt], "dve")
        pz = self.ps(s)
        for k in range(8):
            self.mm(pz[0:nt, 0:64], xT[:, k, 0:nt], w[:, k, 16:80], start=(k == 0), stop=(k == 7))
        self.rope(self.kinew[0:nt, :], pz[0:nt, 0:64], (rt[0:nt, 128:160], rt[0:nt, 160:192]), nt, 1, 32)
        self.dma("sp", ki_out, self.kinew[0:nt, :])
        self.cp(self.kidup[0:nt, 0:64], self.kinew[0:nt, :], "pool")
        self.cp(self.kidup[0:nt, 64:128], self.kinew[0:nt, :], "pool")
        self.tr(pt[:, 0:nt], self.kidup[0:nt, :], self.ident[0:nt, 0:nt])
        self.cp(kv["KI"][:, kt * 128:kt * 128 + nt].rk((kv["n"] + "KI", kt)), pt[:, 0:nt], "dve")
        self.mm(pg[0:nt, :], self.grx[0:17, 0:nt], self.wup[0:17, :])
        self.act(d["e1"][0:nt, :], pg[0:nt, :], AF.Exp, scale=-1.0)
        self.act(d["L"][0:nt, :], d["e1"][0:nt, :], AF.Ln, bias=1.0)
        self.mm(pg[0:nt, :], self.uneg[0:nt, 0:nt], d["L"][0:nt, :])
        self.act(d["En"][0:nt, :], pg[0:nt, :], AF.Exp, scale=-1.0)
        for h in range(4):
            self.mm(pg[:, h * nt:(h + 1) * nt], d["L"][0:nt, h * 128:(h + 1) * 128], self.uneg[0:nt, 0:nt])
        self.act(d["EpT"][:, :, 0:nt], pg[:, 0:4 * nt].rr("p (h t) -> p h t", h=4), AF.Exp)

    def sh_gk(self, s, nt, w):
        d = self.sl[s]
        xT = d["xT"]
        pz = self.ps(s)
        pt = self.ps(3, BF16)
        for k in range(8):
            self.mm(pz[0:nt, :], xT[:, k, 0:nt], w[:, k, :], start=(k == 0), stop=(k == 7))
        self.tt(d["Ktm"][0:nt, :], pz[0:nt, :], d["En"][0:nt, :], ALU.mult)
        for h in range(4):
            self.tr(pt[:, h * nt:(h + 1) * nt], d["Ktm"][0:nt, h * 128:(h + 1) * 128], self.ident[0:nt, 0:nt])
        self.cp(d["KtT"][:, :, 0:nt], pt[:, 0:4 * nt].rr("p (h t) -> p h t", h=4), "dve")

    def sh_gv(self, s, nt, w, c):
        d = self.sl[s]
        pz = self.ps(s)
        for k in range(8):
            self.mm(pz[0:nt, :], d["xT"][:, k, 0:nt], w[:, k, :], start=(k == 0), stop=(k == 7))
        self.cp(d["Vg"][0:nt, c * 512:(c + 1) * 512], pz[0:nt, :], "dve")

    def sh_dkv(self, s, nt, w, kt, k_out, v_out, kv=None):
        kv = kv or self.kvp
        d = self.sl[s]
        rt = self.rt[s]
        pz = self.ps(s)
        pt = self.ps(3, BF16)
        for k in range(8):
            self.mm(pz[0:nt, :], d["xT"][:, k, 0:nt], w[:, k, :], start=(k == 0), stop=(k == 7))
        self.rope(self.knew[0:nt, :], pz[0:nt, 0:256], (rt[0:nt, 0:64], rt[0:nt, 64:128]), nt, 2, 64)
        self.cp(self.vnew[0:nt, :], pz[0:nt, 256:512], "dve")
        self.cp(kv["VX"][0:nt, kt, :, 0:128].rk((kv["n"] + "VX", kt)), pz[0:nt, 256:512].rr("p (g d) -> p g d", g=2), "dve")
        self.dma("sp", k_out, self.knew[0:nt, :])
        self.dma("sp", v_out, self.vnew[0:nt, :])
        self.cp(self.kb16[0:nt, :], self.knew[0:nt, :], "pool")
        for g in range(2):
            self.tr(pt[:, g * nt:(g + 1) * nt], self.kb16[0:nt, g * 128:(g + 1) * 128], self.ident[0:nt, 0:nt])
        self.cp(kv["KT"][:, :, kt * 128:kt * 128 + nt].rk((kv["n"] + "KT", kt)), pt[:, 0:2 * nt].rr("p (g t) -> p g t", g=2), "dve")

    def sh_state(self, s, nt):
        d = self.sl[s]
        self.cp(d["Sbf"], self.S, "pool")
        for hp in range(2):
            pst = self.ps(hp)
            for hh in range(2):
                h = 2 * hp + hh
                self.mm(pst[:, hh * 256:(hh + 1) * 256], d["Ktm"][0:nt, h * 128:(h + 1) * 128],
                        d["Vg"][0:nt, h * 256:(h + 1) * 256])
            for hh in range(2):
                h = 2 * hp + hh
                el = d["EpT"][:, h, nt - 1:nt]
                self.ts(self.S[:, h, :], self.S[:, h, :], el, ALU.mult)
                self.stt(self.S[:, h, :], pst[:, hh * 256:(hh + 1) * 256], el, self.S[:, h, :], ALU.mult, ALU.add)

    def prefetch_x(self, j):
        I = self.inp
        for s in range(2):
            r0 = (2 * j + s) * 128
            self.dma("sp", self.xs[s][0:128, :], I["x_seq"][r0:r0 + 128, :])
            self.dma("sp", self.rt[s][0:128, :], I["rope_seq"][r0:r0 + 128, :])
        if self.stage >= 2:
            b = j % 2
            self.dma("sp", self.xo2[j % 3][0:128, :], I["x_own"][j * 128:(j + 1) * 128, :])
            self.dma("sp", self.rto2[b][0:128, :], I["rope_own"][j * 128:(j + 1) * 128, :])

    def g_shared(self, j):
        I, O = self.inp, self.out
        tiles = []
        for s in range(2):
            i = 2 * j + s
            r0 = i * 128
            self.load_xT(None, 128, self.sl[s]["xT"], self.xs[s], trb=3, load=False)
            tiles.append((s, i, r0))
            yield 6.0
        b = j % 2
        if self.stage >= 2:
            self.load_xT(None, 128, self.xoT2[j % 3], self.xo2[j % 3], trb=3, load=False)
            self.flag_xo = j
            yield 6.0
        w = self.wget("sm")
        for s, i, r0 in tiles:
            self.sh_sm(s, 128, w, i, O["ki_p"][r0:r0 + 128, :])
        if self.stage >= 2:
            self.own_wi(w, self.xoT2[j % 3], 128, self.wi2[b])
        self.flag_ki = j
        yield 14.0
        w = self.wget("gk")
        for s, i, r0 in tiles:
            self.sh_gk(s, 128, w)
        yield 10.0
        for c in range(2):
            w = self.wget(f"gv{c}")
            for s, i, r0 in tiles:
                self.sh_gv(s, 128, w, c)
            yield 8.0
        w = self.wget("dkv")
        for s, i, r0 in tiles:
            self.sh_dkv(s, 128, w, i, O["k_p"][r0:r0 + 128, :], O["v_p"][r0:r0 + 128, :])
        yield 14.0
        for s, i, r0 in tiles:
            self.sh_state(s, 128)
            yield 6.0

    def run(self, gen):
        for _ in gen:
            pass

    def interleave(self, gens, until=None):
        until = list(range(len(gens))) if until is None else until
        while not all(gens[k][2] for k in until):
            act = [k for k in range(len(gens)) if not gens[k][2]]
            k = min(act, key=lambda q: gens[q][1])
            try:
                c = next(gens[k][0])
                if c == 0.0:
                    others = [gens[q][1] for q in act if q != k]
                    gens[k][1] = (max(others) if others else gens[k][1]) + 1e-3
                else:
                    gens[k][1] += (c or 0.0) * (gens[k][3] if len(gens[k]) > 3 else 1.0)
            except StopIteration:
                gens[k][2] = True

    def pair_args(self, j):
        b = j % 2
        return dict(nt=128, L=256 * (j + 1), xoT=self.xoT2[j % 3], xo=self.xo2[j % 3], rto=self.rto2[b], KtT=self.KtT_o,
                    Vg=self.Vg_o, EpT=self.EpT_o, Sbf=self.Sbf_o, admb=self.admb, slot=j, wi=self.wi2[b], par=b)

    def g_pre1(self, j):
        yield from self.g_shared(j)
        if self.stage < 2:
            return
        m0, m1 = self.pm[:, 0:1], self.pm[:, 1:2]
        for name, dst in (("KtT", self.KtT_o), ("Vg", self.Vg_o), ("EpT", self.EpT_o), ("Sbf", self.Sbf_o)):
            self.ts(dst, self.sl[0][name], m0, ALU.mult)
            self.stt(dst, self.sl[1][name], m1, dst, ALU.mult, ALU.add)
        yield 6.0
        yield from self.g_front_gla(**self.pair_args(j))

    def g_pre2(self, j, wait=None):
        if self.stage < 2:
            return
        while self.flag_xo < j:
            yield 0.0
        w2 = (lambda: self.flag_ki >= j and (wait is None or wait()))
        yield from self.g_front_q(wait=w2, **self.pair_args(j))

    def prompt(self):
        self.flag_xo = -1
        self.flag_ki = -1
        self.prefetch_x(0)
        self.bisect_done = True
        self.interleave([[self.g_pre1(0), 0.0, False], [self.g_pre2(0), 0.0, False]])
        back = None
        for j in range(NPAIR):
            if j + 1 < NPAIR:
                self.prefetch_x(j + 1)
            if self.stage < 2:
                if j + 1 < NPAIR:
                    self.run(self.g_pre1(j + 1))
                continue
            if j >= 1 and self.we_chunks:
                c = self.we_chunks.pop(0)
                a_, b_ = c * self.CH, min(WE_TOT, (c + 1) * self.CH)
                self.dma("pool", self.we16[:, a_:b_].rk(("we16", c)), self.inp["we"][:, a_:b_])
            args = self.pair_args(j)
            self.bisect_done = False
            self.attn_done = False
            self.back_done = False
            if j + 1 < NPAIR:
                B1 = [self.g_pre1(j + 1), 0.0, False]
                B2 = [self.g_pre2(j + 1, wait=lambda: self.bisect_done and self.attn_done), 0.0, False]
            elif self.stage >= 3:
                self.dma("sp", self.out["st_p"].rr("h k v -> k h v"), self.S)
                self.flag_sx = False
                self.flag_sk = False
                B1 = [self.g_sample_pre1(), 0.0, False]
                B2 = [self.g_sample_pre2(lambda: self.bisect_done and self.attn_done), 0.0, False]
            else:
                B1 = [iter(()), 0.0, False]
                B2 = [iter(()), 0.0, False]
            s1 = [[self.g_bisect(**args), 0.0, False], [back if back is not None else iter(()), 0.0, False], B1, B2]
            B1.append(BSCALE)
            B2.append(BSCALE)
            self.interleave(s1, until=[0, 1])
            B1[3] = 1.3
            B2[3] = 1.3
            self.back_done = True
            B1[1] = 0.0
            B2[1] = 0.0
            s2 = [[self.g_attn(**args), 0.0, False], B1, B2]
            self.interleave(s2)
            back = self.g_back(**args)
        if self.stage >= 3:
            sargs = self.sample_args()
            self.interleave([[self.g_bisect(**sargs), 0.0, False], [back, 0.0, False]])
            self.run(self.g_attn(**sargs))
            self.run(self.g_back(**sargs))
        elif back is not None:
            self.run(back)

    def own_wi(self, wsm, xoT, nt, wi):
        pz = self.ps(1)
        for k in range(8):
            self.mm(pz[0:nt, 64:72], xoT[:, k, 0:nt], wsm[:, k, 80:88], start=(k == 0), stop=(k == 7))
        self.ts(wi[0:nt, 0:8], pz[0:nt, 64:72], IDX_W_SCALE, ALU.mult)

    def transposes(self, dstT, src_tm, nt, nblk, bank=2):
        pt = self.ps(bank, BF16)
        for b in range(nblk):
            self.tr(pt[:, b * nt:(b + 1) * nt], src_tm[0:nt, b * 128:(b + 1) * 128], self.ident[0:nt, 0:nt])
        self.cp(dstT[:, 0:nblk, 0:nt], pt[:, 0:nblk * nt].rr("p (b t) -> p b t", b=nblk), "dve")

    def layernorm_stats(self, src, nt, ngrp, width, mv, st, rs):
        for g in range(ngrp):
            self.raw("dve", "bn_stats", [st], [src], out=st[0:nt, g * 6:(g + 1) * 6], in_=src[0:nt, g * width:(g + 1) * width])
        self.raw("dve", "bn_aggr", [mv], [st], out=mv[0:nt, 0:2], in_=st[0:nt, 0:6 * ngrp])
        self.ts(rs[0:nt, 0:1], mv[0:nt, 1:2], LN_EPS, ALU.add, eng="pool")
        self.tt(rs[0:nt, 0:1], rs[0:nt, 0:1], self.cbias(-0.5)[0:nt, :], ALU.pow, eng="pool")

    def g_front_gla(self, nt, L, xoT, xo, rto, KtT, Vg, EpT, Sbf, admb, slot, wi, par=0, wait=None, kv=None):
        kv = kv or self.kvp
        ogT = self.ogT2[par]
        I, O = self.inp, self.out
        sm = self.sm
        w = self.wget("gq")
        pq = self.ps(3)
        for h in range(4):
            for k in range(8):
                self.mm(pq[:, h * nt:(h + 1) * nt], w[:, k, h * 128:(h + 1) * 128], xoT[:, k, 0:nt],
                        start=(k == 0), stop=(k == 7))
        self.stt(self.QtT[:, :, 0:nt], pq[:, 0:4 * nt].rr("p (h t) -> p h t", h=4), DK ** -0.5,
                 EpT[:, :, 0:nt], ALU.mult, ALU.mult)
        yield 4.0
        pa = self.ps(4)
        for h in range(4):
            self.mm(pa[0:nt, h * nt:(h + 1) * nt], KtT[:, h, 0:nt], self.QtT[:, h, 0:nt])
        self.tt(self.AT[0:nt, :, 0:nt], pa[0:nt, 0:4 * nt].rr("p (h t) -> p h t", h=4),
                self.cm[0:nt, 0:nt].uq(1).bt([nt, 4, nt]), ALU.mult)
        for c in range(2):
            w = self.wget(f"gg{c}")
            pz = self.ps(c)
            for k in range(8):
                self.mm(pz[0:nt, :], xoT[:, k, 0:nt], w[:, k, :], start=(k == 0), stop=(k == 7))
            self.act(self.sgg[0:nt, c * 512:(c + 1) * 512], pz[0:nt, :], AF.Silu)
            yield 3.0
        on = self.bufA
        for hp in range(2):
            po = self.ps(hp)
            for hh in range(2):
                h = 2 * hp + hh
                self.mm(po[0:nt, hh * 256:(hh + 1) * 256], self.AT[0:nt, h, 0:nt], Vg[0:nt, h * 256:(h + 1) * 256],
                        start=True, stop=False)
                self.mm(po[0:nt, hh * 256:(hh + 1) * 256], self.QtT[:, h, 0:nt], Sbf[:, h, :],
                        start=False, stop=True)
            for hh in range(2):
                h = 2 * hp + hh
                st, mv, rs = sm[:, 16:22], sm[:, 22:24], sm[:, 24:25]
                self.layernorm_stats(po[:, hh * 256:(hh + 1) * 256], nt, 1, 256, mv, st, rs)
                self.ts(on[0:nt, h * 256:(h + 1) * 256], po[0:nt, hh * 256:(hh + 1) * 256], mv[0:nt, 0:1], ALU.subtract,
                        rs[0:nt, 0:1], ALU.mult)
        on3 = on[0:nt, :].rr("p (h v) -> p h v", h=4)
        self.tt(on3, on3, self.gn[0:nt, 0, :].uq(1).bt([nt, 4, 256]), ALU.mult)
        self.tt(on3, on3, self.gn[0:nt, 1, :].uq(1).bt([nt, 4, 256]), ALU.add)
        og = self.stg[1]
        self.tt(og[0:nt, :], on[0:nt, :], self.sgg[0:nt, :], ALU.mult)
        self.transposes(ogT, og, nt, 8, bank=3)
        yield 18.0

    def g_front_q(self, nt, L, xoT, xo, rto, KtT, Vg, EpT, Sbf, admb, slot, wi, par=0, wait=None, kv=None):
        kv = kv or self.kvp
        sm = self.sm
        qT = self.qT2[par]
        qr = self.stg[2]
        for c in range(2):
            w = self.wget(f"dq{c}")
            pz = self.ps(c)
            for k in range(8):
                self.mm(pz[0:nt, :], xoT[:, k, 0:nt], w[:, k, :], start=(k == 0), stop=(k == 7))
            self.rope(qr[0:nt, c * 512:(c + 1) * 512], pz[0:nt, :], (rto[0:nt, 0:64], rto[0:nt, 64:128]), nt, 4, 64)
            yield 5.0
        self.transposes(qT, qr, nt, 8, bank=3)
        yield 3.0
        w = self.wget("iq")
        pz = self.ps(0)
        for k in range(8):
            self.mm(pz[0:nt, :], xoT[:, k, 0:nt], w[:, k, :], start=(k == 0), stop=(k == 7))
        self.rope(self.qir[0:nt, :], pz[0:nt, :], (rto[0:nt, 128:160], rto[0:nt, 160:192]), nt, 8, 32)
        self.transposes(self.qiT, self.qir, nt, 4, bank=3)
        yield 6.0
        while wait is not None and not wait():
            yield 0.0
        idx = self.idx
        nkb = (L + 511) // 512
        ri = 0
        for kb in range(nkb):
            n = min(512, L - kb * 512)
            kts = MK((kv["n"] + "KI", t) for t in range(kb * 4, (kb * 512 + n + 127) // 128))
            blk = idx[0:nt, kb * 512:kb * 512 + n]
            for hp in range(4):
                pab = (self.ps(0), self.ps(1)) if hp % 2 == 0 else (self.ps(3), self.ps(4))
                for hh in range(2):
                    lo, hi = (0, 64) if hh == 0 else (64, 128)
                    self.mm(pab[hh][0:nt, 0:n], self.qiT[lo:hi, hp, 0:nt],
                            kv["KI"][lo:hi, kb * 512:kb * 512 + n].rk(kts))
                for hh in range(2):
                    h = 2 * hp + hh
                    r = self.rbuf[ri % 2]
                    ri += 1
                    self.act(r[0:nt, 0:n], pab[hh][0:nt, 0:n], AF.Relu)
                    if h == 0:
                        self.ts(blk, r[0:nt, 0:n], wi[0:nt, 0:1], ALU.mult)
                    else:
                        self.stt(blk, r[0:nt, 0:n], wi[0:nt, h:h + 1], blk, ALU.mult, ALU.add)
                yield 1.3 * n / 512

    def g_bisect(self, nt, L, xoT, xo, rto, KtT, Vg, EpT, Sbf, admb, slot, wi, par=0, wait=None, kv=None):
        kv = kv or self.kvp
        sm = self.sm
        idx = self.idx
        amax, lo_, mid, u = sm[:, 32:33], sm[:, 33:34], sm[:, 34:35], sm[:, 35:36]
        wk = sm[:, 40:40 + NIT + 1]
        sa = sm[:, 64:64 + NIT]
        if L > TOPK:
            self.raw("dve", "tensor_reduce", [amax], [idx], out=amax[0:nt, :], in_=idx[0:nt, 0:L], axis=AX.X,
                     op=ALU.max, apply_absolute_value=True)
        if admb is not None:
            self.tt(idx[0:nt, L - 256:L], idx[0:nt, L - 256:L], admb[0:nt, :], ALU.add)
        if L > TOPK:
            c0 = float(L - 2 * TOPK) + 0.5
            self.ts(wk[0:nt, :], self.pow2[0:nt, 0:NIT + 1], amax[0:nt, :], ALU.mult, 2.0002, ALU.mult)
            self.tt(mid[0:nt, :], wk[0:nt, 0:1], amax[0:nt, :], ALU.subtract)
            yield 3.0
            for k in range(NIT):
                self.act(self.mb[0:nt, 0:L], idx[0:nt, 0:L], AF.Sign, bias=mid[0:nt, :], scale=-1.0,
                         accum=sa[0:nt, k:k + 1])
                self.act(u[0:nt, :], sa[0:nt, k:k + 1], AF.Sign, bias=self.cbias(c0)[0:nt, :], scale=-1.0)
                self.act(mid[0:nt, :], u[0:nt, :], AF.Identity, bias=mid[0:nt, :], scale=wk[0:nt, k + 1:k + 2])
                yield 1.1 + L / 1200.0
            self.tt(lo_[0:nt, :], mid[0:nt, :], wk[0:nt, NIT:NIT + 1], ALU.subtract)
        else:
            self.memset(lo_[0:nt, :], -1e29)
        self.ts(self.mb[0:nt, 0:L], idx[0:nt, 0:L], lo_[0:nt, :], ALU.is_lt, NEG, ALU.mult)
        self.bisect_done = True
        yield 1.0 + L / 1900.0

    def cbias(self, val):
        if not hasattr(self, "_cb"):
            self._cb = {}
            self._cbt = self.cbt
            self._cbn = 0
        if val not in self._cb:
            t = self._cbt[:, self._cbn:self._cbn + 1]
            self._cbn += 1
            assert self._cbn <= 32
            self.memset(t, val, "pool")
            self._cb[val] = t
        return self._cb[val]

    def g_attn(self, nt, L, xoT, xo, rto, KtT, Vg, EpT, Sbf, admb, slot, wi, par=0, wait=None, kv=None):
        kv = kv or self.kvp
        qT_ = self.qT2[par]
        sm = self.sm
        od = self.stg[0]
        nkt = (L + 127) // 128
        seq = [(g, kt) for g in range(2) for kt in range(nkt)]

        def scores(n):
            g, kt = seq[n]
            nk = min(128, L - kt * 128)
            pS = self.ps(2 if n % 2 == 0 else 5)
            ST = pS[0:nk, 0:4 * nt].rr("p (h t) -> p h t", h=4)
            self.mm(ST, kv["KT"][:, g, kt * 128:kt * 128 + nk].rk((kv["n"] + "KT", kt)), qT_[:, 4 * g:4 * g + 4, 0:nt],
                    start=True, stop=False)
            self.mm(ST, self.mb[0:nt, kt * 128:kt * 128 + nk], self.i4[0:nt, :, 0:nt], start=False, stop=True)
            return ST, nk

        cur = scores(0)
        for n, (g, kt) in enumerate(seq):
            ST, nk = cur
            if n + 1 < len(seq):
                cur = scores(n + 1)
            PT = self.PTb[n % 3]
            self.act(PT[0:nk, :, 0:nt], ST, AF.Exp, scale=ATT_SCALE)
            for hh in range(4):
                ob = self.ps(6)[0:nt, hh * 130:hh * 130 + 129] if hh < 3 else self.ps(7)[0:nt, 0:129]
                self.mm(ob, PT[0:nk, hh, 0:nt], kv["VX"][0:nk, kt, g, 0:129].rk(MK(((kv["n"] + "VX", kt), kv["n"] + "VXones"))),
                        start=(kt == 0 and hh in (0, 3)), stop=(kt == nkt - 1), skip=True)
            if kt == nkt - 1:
                for hh in range(4):
                    ob = self.ps(6)[0:nt, hh * 130:hh * 130 + 129] if hh < 3 else self.ps(7)[0:nt, 0:129]
                    rd = sm[:, 36 + hh:37 + hh]
                    self.raw("dve", "reciprocal", [rd], [ob], out=rd[0:nt, :], in_=ob[:, 128:129])
                    hq = 4 * g + hh
                    self.ts(od[0:nt, hq * 128:(hq + 1) * 128], ob[:, 0:128], rd[0:nt, :], ALU.mult)
            if n == len(seq) - 1:
                self.attn_done = True
            yield 1.7

    def g_back(self, nt, L, xoT, xo, rto, KtT, Vg, EpT, Sbf, admb, slot, wi, par=0, wait=None, kv=None):
        kv = kv or self.kvp
        ogT_ = self.ogT2[par]
        if slot < 16:
            xo = self.xres
            self.dma("sp", xo[0:nt, :], self.inp["x_own"][slot * 128:(slot + 1) * 128, :])
        I, O = self.inp, self.out
        sm = self.sm
        od = self.stg[0]
        self.transposes(self.odT, od, nt, 8, bank=6)
        yield 3.0
        m = self.stg[0]
        sa, tb = self.sab, self.tbb
        for c in range(2):
            w = self.wget(f"ga{c}")
            pz = self.ps(2)
            for k in range(8):
                self.mm(pz[0:nt, :], xoT[:, k, 0:nt], w[:, k, :], start=(k == 0), stop=(k == 7))
            self.act(sa[0:nt, :], pz[0:nt, :], AF.Sigmoid)
            w = self.wget(f"wg{c}")
            pz = self.ps(5)
            for k in range(8):
                self.mm(pz[0:nt, :], ogT_[:, k, 0:nt], w[:, k, :], start=(k == 0), stop=(k == 7))
            self.tt(tb[0:nt, :], pz[0:nt, :], sa[0:nt, :], ALU.mult)
            yield 4.0
            w = self.wget(f"gb{c}")
            pz = self.ps(2)
            for k in range(8):
                self.mm(pz[0:nt, :], xoT[:, k, 0:nt], w[:, k, :], start=(k == 0), stop=(k == 7))
            self.act(sa[0:nt, :], pz[0:nt, :], AF.Sigmoid)
            w = self.wget(f"wd{c}")
            pz = self.ps(5)
            for k in range(8):
                self.mm(pz[0:nt, :], self.odT[:, k, 0:nt], w[:, k, :], start=(k == 0), stop=(k == 7))
            self.tt(sa[0:nt, :], pz[0:nt, :], sa[0:nt, :], ALU.mult)
            self.tt(m[0:nt, c * 512:(c + 1) * 512], sa[0:nt, :], tb[0:nt, :], ALU.add)
            yield 4.0
        self.transposes(self.mT, m, nt, 8, bank=6)
        yield 3.0
        v1 = xo
        for c in range(2):
            w = self.wget(f"wo{c}")
            pz = self.ps(2 if c == 0 else 5)
            for k in range(8):
                self.mm(pz[0:nt, :], self.mT[:, k, 0:nt], w[:, k, :], start=(k == 0), stop=(k == 7))
            self.stt(v1[0:nt, c * 512:(c + 1) * 512], xo[0:nt, c * 512:(c + 1) * 512], ALPHA, pz[0:nt, :],
                     ALU.mult, ALU.add)
        yield 4.0
        st, mv, rs = sm[:, 84:96], sm[:, 96:98], sm[:, 98:99]
        self.layernorm_stats(v1, nt, 2, 512, mv, st, rs)
        self.ts(v1[0:nt, :], v1[0:nt, :], mv[0:nt, 0:1], ALU.subtract, rs[0:nt, 0:1], ALU.mult)
        self.dma("sp", self.hscr[slot * 128:slot * 128 + nt, :].rk(("hscr", slot)), v1[0:nt, :])
        if self.stage == 2 or self.stage == 3:
            dst = O["y_own"][slot * 128:(slot + 1) * 128, :] if slot < 16 else O["y_smp"]
            self.dma("sp", dst, v1[0:nt, :])

    def sample_args(self):
        d = self.sl[0]
        return dict(nt=DEC_T, L=PAST + DEC_T, xoT=d["xT"], xo=self.xo2[0], rto=self.rt[0], KtT=d["KtT"], Vg=d["Vg"],
                    EpT=d["EpT"], Sbf=d["Sbf"], admb=None, slot=16, wi=self.wi2[0], par=0, kv=self.kvs)

    def g_sample_pre1(self):
        I, O = self.inp, self.out
        nt = DEC_T
        kv = self.kvs
        pt = self.ps(3, BF16)
        cstage = self.xs[0].bc(BF16)[:, 0:2048].rr("p (t c) -> p t c", t=8)
        kistage = self.xs[1].bc(BF16)[:, 0:1024].rr("p (t c) -> p t c", t=8)
        self.memset(kv["VX"][:, :, :, 128:130].rk("sVXones"), 1.0, "pool")
        self.dma("pool", cstage, I["ck"].rr("(t p) c -> p t c", p=128))
        self.dma("pool", kistage[:, :, 0:64], I["cki"].rr("(t p) c -> p t c", p=128))
        self.dma("pool", kistage[:, :, 64:128], I["cki"].rr("(t p) c -> p t c", p=128))
        for g in range(2):
            self.dma("pool", kv["VX"][:, 0:8, g, 0:128].rk(MK(("sVX", t) for t in range(8))),
                     I["cv"][:, g * 128:(g + 1) * 128].rr("(t p) d -> p t d", p=128))
        self.dma("sp", self.S, I["s0"].rr("h k v -> k h v"))
        d = self.sl[0]
        self.load_xT(I["x_smp"], nt, d["xT"], self.xo2[0], trb=3)
        self.dma("sp", self.rt[0][0:nt, :], I["rope_seq"][PAST:PAST + nt, :])
        self.flag_sx = True
        yield 8.0
        for kt in range(8):
            for g in range(2):
                self.tr(pt[:, g * 128:(g + 1) * 128], cstage[:, kt, g * 128:(g + 1) * 128], self.ident)
            self.tr(pt[:, 256:384], kistage[:, kt, :], self.ident)
            self.cp(kv["KT"][:, :, kt * 128:(kt + 1) * 128].rk(("sKT", kt)), pt[:, 0:256].rr("p (g t) -> p g t", g=2), "dve")
            self.cp(kv["KI"][:, kt * 128:(kt + 1) * 128].rk(("sKI", kt)), pt[:, 256:384], "dve")
            yield 3.0
        w = self.wget("sm")
        self.sh_sm(0, nt, w, 8, O["ki_s"], kv=kv)
        self.own_wi(w, d["xT"], nt, self.wi2[0])
        self.flag_sk = True
        yield 12.0
        self.sh_gk(0, nt, self.wget("gk"))
        yield 5.0
        for c in range(2):
            self.sh_gv(0, nt, self.wget(f"gv{c}"), c)
            yield 4.0
        self.sh_dkv(0, nt, self.wget("dkv"), 8, O["k_s"], O["v_s"], kv=kv)
        yield 7.0
        self.sh_state(0, nt)
        self.dma("sp", O["st_s"].rr("h k v -> k h v"), self.S)
        yield 6.0
        while not self.back_done:
            yield 0.0
        yield from self.g_front_gla(**self.sample_args())

    def g_sample_pre2(self, wait):
        while not self.flag_sx:
            yield 0.0
        yield from self.g_front_q(wait=lambda: self.flag_sk and wait(), **self.sample_args())


    def phase2(self):
        I, O = self.inp, self.out
        while self.we_chunks:
            c = self.we_chunks.pop(0)
            a_, b_ = c * self.CH, min(WE_TOT, (c + 1) * self.CH)
            self.dma("pool", self.we16[:, a_:b_].rk(("we16", c)), I["we"][:, a_:b_])
        self.prog.barrier()
        ar = self.ar
        ar.off = self.p2_mark
        A = ar.alloc
        NS = 17
        slots = [(s, 128) for s in range(16)] + [(16, DEC_T)]
        hacc = A([NS, 1024], F32)
        hT = A([8, NS * 128], BF16)
        hb = A([1024], BF16)
        wr = A([8, 20], BF16)
        self.dma("pool", wr, I["wr"].rr("p (k c) -> p k c", k=8))
        brt = A([20], F32)
        self.dma("sp", brt, Buf(I["br"].ap.partition_broadcast(128), None))
        ln1 = A([2, 1024], F32)
        self.dma("sp", ln1[:, 0, :], Buf(I["ln"].ap[0:1, :].partition_broadcast(128), None))
        self.dma("sp", ln1[:, 1, :], Buf(I["ln"].ap[1:2, :].partition_broadcast(128), None))
        ln2 = A([2, 1024], F32)
        self.dma("sp", ln2[:, 0, :], Buf(I["ln"].ap[2:3, :].partition_broadcast(128), None))
        self.dma("sp", ln2[:, 1, :], Buf(I["ln"].ap[3:4, :].partition_broadcast(128), None))
        sel = A([16 * 128], F32, parts=16)
        self.dma("sp", sel, I["sel"])
        gateT = A([NS * 128], F32, parts=16)
        sm = A([128], F32)
        NR = 3
        ering = [A([WE_PER], BF16, key=("ering", i)) for i in range(NR)]
        sgb = A([2, 512], BF16)
        gbs = A([512], F32)
        tb = A([2, 512], BF16)
        EG = 2
        hid = [A([2, 512], BF16) for _ in range(EG)]
        print("phase-2 SBUF bytes/partition:", ar.off)
        pt = self.ps(2, BF16)
        for s, nt in slots:
            hs = hacc[:, s, :]
            self.dma("sp", hs[0:nt, :], self.hscr[s * 128:s * 128 + nt, :].rk(("hscr", s)))
            self.tt(hs[0:nt, :], hs[0:nt, :], ln1[0:nt, 0, :], ALU.mult)
            self.tt(hs[0:nt, :], hs[0:nt, :], ln1[0:nt, 1, :], ALU.add)
            self.cp(hb[0:nt, :], hs[0:nt, :], "act")
            for k in range(8):
                self.tr(pt[:, k * nt:(k + 1) * nt], hb[0:nt, k * 128:(k + 1) * 128], self.ident[0:nt, 0:nt])
            self.cp(hT[:, :, s * 128:s * 128 + nt], pt[:, 0:8 * nt].rr("p (k t) -> p k t", k=8), "dve")
            pz = self.ps(0)
            for k in range(8):
                self.mm(pz[0:nt, 0:20], hT[:, k, s * 128:s * 128 + nt], wr[:, k, :], start=(k == 0), stop=(k == 7))
            lg = sm[:, 0:20]
            self.tt(lg[0:nt, :], pz[0:nt, 0:20], brt[0:nt, :], ALU.add)
            gmax, nge, sume, pg = sm[:, 20:21], sm[:, 21:22], sm[:, 22:23], sm[:, 23:24]
            goh = sm[:, 24:28]
            em = sm[:, 32:48]
            oh1 = sm[:, 48:64]
            oh2 = sm[:, 64:80]
            m1, m2, e2, w1, w2 = sm[:, 80:81], sm[:, 81:82], sm[:, 82:83], sm[:, 83:84], sm[:, 84:85]
            junk = sm[:, 88:92]
            gate = sm[:, 96:112]
            self.raw("dve", "tensor_reduce", [gmax], [lg], out=gmax[0:nt, :], in_=lg[0:nt, 0:4], axis=AX.X, op=ALU.max)
            self.ts(nge[0:nt, :], gmax[0:nt, :], -1.0, ALU.mult)
            self.act(junk[0:nt, :], lg[0:nt, 0:4], AF.Exp, bias=nge[0:nt, :], accum=sume[0:nt, :])
            self.raw("dve", "reciprocal", [pg], [sume], out=pg[0:nt, :], in_=sume[0:nt, :])
            self.ts(goh[0:nt, :], lg[0:nt, 0:4], gmax[0:nt, :], ALU.is_ge, 1.0, ALU.subtract)
            self.ts(goh[0:nt, :], goh[0:nt, :], 1e30, ALU.mult)
            self.tt(em[0:nt, :].rr("p (g e) -> p g e", g=4), lg[0:nt, 4:20].rr("p (g e) -> p g e", g=4),
                    goh[0:nt, :].uq(2).bt([nt, 4, 4]), ALU.add)
            self.raw("dve", "tensor_reduce", [m1], [em], out=m1[0:nt, :], in_=em[0:nt, :], axis=AX.X, op=ALU.max)
            self.ts(oh1[0:nt, :], em[0:nt, :], m1[0:nt, :], ALU.is_ge)
            self.stt(em[0:nt, :], oh1[0:nt, :], -1e30, em[0:nt, :], ALU.mult, ALU.add)
            self.raw("dve", "tensor_reduce", [m2], [em], out=m2[0:nt, :], in_=em[0:nt, :], axis=AX.X, op=ALU.max)
            self.ts(oh2[0:nt, :], em[0:nt, :], m2[0:nt, :], ALU.is_ge)
            self.ts(nge[0:nt, :], m1[0:nt, :], -1.0, ALU.mult)
            self.act(e2[0:nt, :], m2[0:nt, :], AF.Exp, bias=nge[0:nt, :])
            self.ts(w1[0:nt, :], e2[0:nt, :], 1.0, ALU.add)
            self.raw("dve", "reciprocal", [w1], [w1], out=w1[0:nt, :], in_=w1[0:nt, :])
            self.tt(w1[0:nt, :], w1[0:nt, :], pg[0:nt, :], ALU.mult)
            self.tt(w2[0:nt, :], w1[0:nt, :], e2[0:nt, :], ALU.mult)
            self.ts(gate[0:nt, :], oh1[0:nt, :], w1[0:nt, :], ALU.mult)
            self.stt(gate[0:nt, :], oh2[0:nt, :], w2[0:nt, :], gate[0:nt, :], ALU.mult, ALU.add)
            pg_ = self.ps(1)
            self.tr(pg_[0:16, 0:nt], gate[0:nt, :], self.identf[0:nt, 0:nt])
            self.cp(gateT[0:16, s * 128:s * 128 + nt], pg_[0:16, 0:nt], "act")
            self.ts(hs[0:nt, :], hs[0:nt, :], ALPHA, ALU.mult)
        batches = [(b * 512, 512, [(4 * b + i, i * 128, 128) for i in range(4)]) for b in range(4)]
        batches.append((2048, DEC_T, [(16, 0, DEC_T)]))
        issued = 0

        def eload(upto):
            nonlocal issued
            while issued < min(16, upto):
                e = issued
                o0 = e * WE_PER
                chs = MK(("we16", q) for q in range(o0 // self.CH, (o0 + WE_PER - 1) // self.CH + 1))
                self.dma("sp", ering[e % NR], self.we16[:, o0:o0 + WE_PER].rk(chs))
                issued += 1

        pd = 0
        for g0 in range(0, 16, EG):
            eload(g0 + NR)
            ws = []
            for e in range(g0, g0 + EG):
                r = ering[e % NR]
                ws.append((r[:, 0:4096].rr("p (k c) -> p k c", k=8), r[:, 4096:WE_PER].rr("p (f c) -> p f c", f=2)))
            for c0, N, bslots in batches:
                for ei in range(EG):
                    e = g0 + ei
                    wgu, wd = ws[ei]
                    for fc in range(2):
                        pG, pU = self.ps(fc), self.ps(2 + fc)
                        for k in range(8):
                            self.mm(pG[:, 0:N], wgu[:, k, fc * 128:(fc + 1) * 128], hT[:, k, c0:c0 + N],
                                    start=(k == 0), stop=(k == 7))
                        for k in range(8):
                            self.mm(pU[:, 0:N], wgu[:, k, 256 + fc * 128:256 + (fc + 1) * 128], hT[:, k, c0:c0 + N],
                                    start=(k == 0), stop=(k == 7))
                    pB = self.ps(4)
                    self.mm(pB[:, 0:N], sel[0:16, e * 128:(e + 1) * 128], gateT[0:16, c0:c0 + N])
                    for fc in range(2):
                        self.act(sgb[:, fc, 0:N], self.ps(fc)[:, 0:N], AF.Silu)
                    self.cp(gbs[:, 0:N], pB[:, 0:N], "act")
                    for fc in range(2):
                        self.tt(tb[:, fc, 0:N], self.ps(2 + fc)[:, 0:N], sgb[:, fc, 0:N], ALU.mult)
                    self.tt(hid[ei][:, :, 0:N], tb[:, :, 0:N], gbs[:, 0:N].uq(1).bt([128, 2, N]), ALU.mult, eng="pool")
                for s, off, nt in bslots:
                    for c in range(2):
                        pD = self.ps(5 + pd % 3)
                        pd += 1
                        n_acc = 2 * EG
                        a = 0
                        for ei in range(EG):
                            for fc in range(2):
                                self.mm(pD[0:nt, :], hid[ei][:, fc, off:off + nt], ws[ei][1][:, fc, c * 512:(c + 1) * 512],
                                        start=(a == 0), stop=(a == n_acc - 1))
                                a += 1
                        self.tt(hacc[0:nt, s, c * 512:(c + 1) * 512], pD[0:nt, :], hacc[0:nt, s, c * 512:(c + 1) * 512], ALU.add)
        for s, nt in slots:
            hs = hacc[:, s, :]
            st, mv, rs = sm[:, 0:12], sm[:, 12:14], sm[:, 14:15]
            self.layernorm_stats(hs, nt, 2, 512, mv, st, rs)
            self.ts(hs[0:nt, :], hs[0:nt, :], mv[0:nt, 0:1], ALU.subtract, rs[0:nt, 0:1], ALU.mult)
            self.tt(hs[0:nt, :], hs[0:nt, :], ln2[0:nt, 0, :], ALU.mult)
            self.tt(hs[0:nt, :], hs[0:nt, :], ln2[0:nt, 1, :], ALU.add)
            dst = O["y_own"][s * 128:(s + 1) * 128, :] if s < 16 else O["y_smp"]
            self.dma("sp", dst, hs[0:nt, :])


def _rope_table(pos):
    pos = np.asarray(pos, np.float32)
    out = np.zeros((len(pos), 192), np.float32)
    for half, c0 in ((64, 0), (32, 128)):
        inv = (10000.0 ** (-np.arange(half, dtype=np.float32) / half)).astype(np.float32)
        ang = pos[:, None] * inv[None, :]
        out[:, c0:c0 + half] = np.cos(ang)
        out[:, c0 + half:c0 + 2 * half] = np.sin(ang)
    return out


def _consts(p):
    c = np.zeros((128, C_TOT), np.float32)
    r = np.arange(128)
    c[:, _C["ident"][0]:_C["ident"][1]] = np.eye(128, dtype=np.float32)
    tri = (r[:, None] <= r[None, :]).astype(np.float32)
    c[:, _C["uneg"][0]:_C["uneg"][1]] = tri * (-1.0 / 16.0)
    c[:, _C["cm"][0]:_C["cm"][1]] = tri
    c[:, _C["pow2"][0]:_C["pow2"][1]] = (0.5 ** (np.arange(32) + 1))[None, :]
    c[:, _C["pm"][0]] = 1.0 - p
    c[:, _C["pm"][0] + 1] = float(p)
    rr = r + 128 * p
    lim = (rr // 64 + 1) * 64
    s = np.arange(256)
    c[:, _C["admb"][0]:_C["admb"][1]] = np.where(s[None, :] < lim[:, None], 0.0, -1e30)
    return c


def _pack_blocks(w_in, wbg, wbd, wo):
    src = {"in": w_in, "bg": wbg, "bd": wbd, "wo": wo}
    out = np.empty((128, WA_TOT), np.float32)
    for n, parts in _BLK:
        m = np.concatenate([src[s][:, a:b] for s, a, b in parts], axis=1)
        c = m.shape[1]
        out[:, _BLK_OFF[n]:_BLK_OFF[n] + 8 * c] = m.reshape(8, 128, c).transpose(1, 0, 2).reshape(128, 8 * c)
    return out


def _pack_experts(wg, wu, wd):
    out = np.empty((128, WE_TOT), np.float32)
    for e in range(16):
        gu = np.concatenate([wg[e], wu[e]], axis=1)
        o = e * WE_PER
        out[:, o:o + 4096] = gu.reshape(8, 128, 512).transpose(1, 0, 2).reshape(128, 4096)
        out[:, o + 4096:o + WE_PER] = wd[e].reshape(2, 128, 1024).transpose(1, 0, 2).reshape(128, 2048)
    return out


STAGE = 4
_CACHE = {}


def kernel(x_prompt, x_sample, state_gla, cache_k, cache_v, cache_k_idx, w_in, w_gla_gate_up, b_gla_gate,
           gla_norm_g, gla_norm_b, w_branch_gla, w_branch_dsa, w_out, ln1_g, ln1_b, w_router_group,
           b_router_group, w_router_expert, b_router_expert, w_expert_gate, w_expert_up, w_expert_down,
           ln2_g, ln2_b):
    f = lambda a: np.ascontiguousarray(np.asarray(a, dtype=np.float32))
    x_prompt, x_sample = f(x_prompt), f(x_sample)
    wa = _pack_blocks(f(w_in)[0], f(w_branch_gla)[0], f(w_branch_dsa)[0], f(w_out)[0])
    we = _pack_experts(f(w_expert_gate)[0], f(w_expert_up)[0], f(w_expert_down)[0])
    wup = np.concatenate([f(w_gla_gate_up)[0], f(b_gla_gate)], axis=0)
    gn = np.concatenate([f(gla_norm_g), f(gla_norm_b)], axis=0)
    ln = np.concatenate([f(ln1_g), f(ln1_b), f(ln2_g), f(ln2_b)], axis=0)
    wrc = np.concatenate([f(w_router_group)[0]] + [f(w_router_expert)[0, g] for g in range(4)], axis=1)
    wr = wrc.reshape(8, 128, 20).transpose(1, 0, 2).reshape(128, 160)
    br = np.concatenate([f(b_router_group)[0], f(b_router_expert)[0].reshape(-1)])[None, :]
    sel = np.zeros((16, 16, 128), np.float32)
    for e in range(16):
        sel[e, e, :] = 1.0
    sel = sel.reshape(16, 2048)
    rope_all = _rope_table(np.arange(SEQ))
    in_maps = []
    for c in range(8):
        b, p = c // 2, c % 2
        xs = x_prompt[b]
        own_rows = (np.arange(NPAIR)[:, None] * 256 + p * 128 + np.arange(128)[None, :]).reshape(-1)
        in_maps.append({
            "x_seq": xs, "x_own": np.ascontiguousarray(xs[own_rows]), "x_smp": x_sample[c],
            "s0": f(state_gla)[0, c], "ck": f(cache_k)[0, c].reshape(PAST, 256),
            "cv": f(cache_v)[0, c].reshape(PAST, 256), "cki": f(cache_k_idx)[0, c],
            "wa": wa, "we": we, "wup": wup, "gn": gn, "ln": ln, "wr": wr, "br": br, "sel": sel,
            "cst": _consts(p), "rope_seq": rope_all, "rope_own": np.ascontiguousarray(rope_all[own_rows]),
        })
    if "nc" not in _CACHE:
        planner = K(STAGE)
        planner.build()
        _CACHE["nc"] = K(STAGE, wseq=planner.wseq).build()
    res = run_bass_kernel_spmd(_CACHE["nc"], in_maps, core_ids=list(range(8)))
    R = res.results
    y_p = np.zeros((4, SEQ, D), np.float32)
    for c in range(8):
        b, p = c // 2, c % 2
        y_p[b].reshape(NPAIR, 2, 128, D)[:, p] = R[c]["y_own"].reshape(NPAIR, 128, D)
    y_s = np.stack([R[c]["y_smp"] for c in range(8)])
    st_p = np.stack([R[2 * b]["st_p"] for b in range(4)])[None]
    k_p = np.stack([R[2 * b]["k_p"].reshape(SEQ, 2, 128) for b in range(4)])[None]
    v_p = np.stack([R[2 * b]["v_p"].reshape(SEQ, 2, 128) for b in range(4)])[None]
    ki_p = np.stack([R[2 * b]["ki_p"] for b in range(4)])[None]
    st_s = np.stack([R[c]["st_s"] for c in range(8)])[None]
    k_s = np.stack([R[c]["k_s"].reshape(DEC_T, 2, 128) for c in range(8)])[None]
    v_s = np.stack([R[c]["v_s"].reshape(DEC_T, 2, 128) for c in range(8)])[None]
    ki_s = np.stack([R[c]["ki_s"] for c in range(8)])[None]
    return (y_p, y_s, st_p, k_p, v_p, ki_p, st_s, k_s, v_s, ki_s)
```

```python
import contextlib
import numpy as np
import concourse.bass as bass
import concourse.mybir as mybir
from concourse.bass_utils import run_bass_kernel_spmd

F32 = mybir.dt.float32
BF16 = mybir.dt.bfloat16
AF = mybir.ActivationFunctionType
ALU = mybir.AluOpType
AX = mybir.AxisListType

D = 1024
SEQ = 4096
NPAIR = 16
DEC_T = 16
PAST = 1024
DK = 128
DV = 256
NIT = 16
TOPK = 256
NEG = -30000.0
BSCALE = 0.75
ALPHA = 2.0 ** 0.25
IDX_W_SCALE = (8 ** -0.5) * (64 ** -0.5)
LN_EPS = 1e-5
ATT_SCALE = 128 ** -0.5

_SZ = (512, 512, 1024, 1024, 16, 1024, 256, 256, 512, 64, 8, 1024, 1024)
_NM = ("gq", "gk", "gv", "gg", "gr", "dq", "dk", "dv", "iq", "ik", "iw", "ga", "gb")
_OFF = {}
_o = 0
for _n, _s in zip(_NM, _SZ):
    _OFF[_n] = (_o, _o + _s)
    _o += _s

_BLK = [
    ("sm", [("in", *_OFF["gr"]), ("in", *_OFF["ik"]), ("in", *_OFF["iw"])]),
    ("gk", [("in", *_OFF["gk"])]),
    ("gv0", [("in", 1024, 1536)]),
    ("gv1", [("in", 1536, 2048)]),
    ("dkv", [("in", *_OFF["dk"]), ("in", *_OFF["dv"])]),
    ("gq", [("in", *_OFF["gq"])]),
    ("gg0", [("in", 2048, 2560)]),
    ("gg1", [("in", 2560, 3072)]),
    ("dq0", [("in", 3088, 3600)]),
    ("dq1", [("in", 3600, 4112)]),
    ("iq", [("in", *_OFF["iq"])]),
    ("ga0", [("in", 5208, 5720)]),
    ("wg0", [("bg", 0, 512)]),
    ("gb0", [("in", 6232, 6744)]),
    ("wd0", [("bd", 0, 512)]),
    ("ga1", [("in", 5720, 6232)]),
    ("wg1", [("bg", 512, 1024)]),
    ("gb1", [("in", 6744, 7256)]),
    ("wd1", [("bd", 512, 1024)]),
    ("wo0", [("wo", 0, 512)]),
    ("wo1", [("wo", 512, 1024)]),
]
_BLK_COLS = {}
_BLK_OFF = {}
_t = 0
for _n, _parts in _BLK:
    _c = sum(b - a for _, a, b in _parts)
    _BLK_COLS[_n] = _c
    _BLK_OFF[_n] = _t
    _t += 8 * _c
WA_TOT = _t
_BLK_ORDER = [n for n, _ in _BLK]

WE_PER = 8 * 512 + 2 * 1024
WE_TOT = 16 * WE_PER

_C = {}
_t = 0
for _n, _s in (("ident", 128), ("uneg", 128), ("cm", 128), ("pow2", 32), ("pm", 2), ("admb", 256)):
    _C[_n] = (_t, _t + _s)
    _t += _s
C_TOT = _t


class Prog:
    ENGS = ("pe", "act", "dve", "pool", "sp")

    def __init__(self, n_dma=24, n_fresh=40):
        self.n_fresh = n_fresh
        self.fresh_used = 0
        self.ops = {e: [] for e in self.ENGS}
        self.cnt = {e: 0 for e in self.ENGS}
        self.known = {e: {} for e in self.ENGS}
        self.last_w = {}
        self.readers = {}
        self.n_dma = n_dma
        self.dma_cnt = [0] * (n_dma + n_fresh)
        self.rr = 0

    def _collect(self, eng, reads, writes):
        deps = {}

        def add(sk, v):
            if deps.get(sk, 0) < v:
                deps[sk] = v

        for k in reads:
            if k in self.last_w:
                add(*self.last_w[k])
        for k in writes:
            if k in self.last_w:
                add(*self.last_w[k])
            for sk, v in self.readers.get(k, {}).items():
                add(sk, v)
        waits = []
        kn = self.known[eng]
        for sk, v in deps.items():
            if sk == "pe" and eng == "pe":
                continue
            if kn.get(sk, 0) >= v:
                continue
            kn[sk] = v
            waits.append((sk, v))
        return waits

    def _register(self, ev, reads, writes):
        sk, v = ev
        for k in reads:
            r = self.readers.setdefault(k, {})
            if r.get(sk, 0) < v:
                r[sk] = v
        for k in writes:
            self.last_w[k] = ev
            self.readers[k] = {}

    def op(self, eng, fn, reads=(), writes=()):
        waits = self._collect(eng, reads, writes)
        self.cnt[eng] += 1
        ev = (eng, self.cnt[eng])
        self._register(ev, reads, writes)
        self.ops[eng].append((waits, fn, eng))

    def dma(self, q, fn, reads=(), writes=(), fresh=False):
        if fresh:
            k = self.n_dma + self.fresh_used
            self.fresh_used += 1
            assert self.fresh_used <= self.n_fresh
        else:
            k = self.rr % self.n_dma
            self.rr += 1
        waits = self._collect(q, reads, writes)
        sk = ("dma", k)
        if self.dma_cnt[k] > 0 and self.known[q].get(sk, 0) < self.dma_cnt[k]:
            self.known[q][sk] = self.dma_cnt[k]
            waits.append((sk, self.dma_cnt[k]))
        self.dma_cnt[k] += 1
        ev = (sk, self.dma_cnt[k])
        self._register(ev, reads, writes)
        self.ops[q].append((waits, fn, sk))

    def barrier(self):
        snap = dict(self.cnt)
        dsnap = list(self.dma_cnt)
        for e in self.ENGS:
            kn = self.known[e]
            waits = []
            for sk, v in snap.items():
                if v > 0 and sk != e and kn.get(sk, 0) < v:
                    kn[sk] = v
                    waits.append((sk, v))
            for k, v in enumerate(dsnap):
                sk = ("dma", k)
                if v > 0 and kn.get(sk, 0) < v:
                    kn[sk] = v
                    waits.append((sk, v))
            if waits:
                self.ops[e].append((waits, None, None))
        self.last_w = {}
        self.readers = {}


class MK(tuple):
    pass


def _keys(k):
    if k is None:
        return []
    if isinstance(k, MK):
        return list(k)
    return [k]


class Buf:
    __slots__ = ("ap", "key", "ps")

    def __init__(self, ap, key, ps=False):
        self.ap = ap
        self.key = key
        self.ps = ps

    def __getitem__(self, idx):
        return Buf(self.ap[idx], self.key, self.ps)

    def rr(self, s, **kw):
        return Buf(self.ap.rearrange(s, **kw), self.key, self.ps)

    def bc(self, dt):
        return Buf(self.ap.bitcast(dt), self.key, self.ps)

    def uq(self, ax):
        return Buf(self.ap.unsqueeze(ax), self.key, self.ps)

    def bt(self, shape):
        return Buf(self.ap.broadcast_to(list(shape)), self.key, self.ps)

    def rk(self, key):
        return Buf(self.ap, key, self.ps)


class Arena:
    def __init__(self, big, nbytes):
        self.big = big
        self.off = 0
        self.cap = nbytes
        self.n = 0

    def alloc(self, free_shape, dtype, parts=128, key=None):
        esz = 2 if dtype == BF16 else 4
        n = int(np.prod(free_shape))
        nb = (n * esz + 31) // 32 * 32
        assert self.off + nb <= self.cap, f"SBUF arena overflow {self.off + nb} > {self.cap}"
        ap = self.big[0:parts, self.off // 4:(self.off + nb) // 4]
        if dtype == BF16:
            ap = ap.bitcast(BF16)
        ap = ap[:, 0:n]
        if len(free_shape) == 2:
            ap = ap.rearrange("p (a b) -> p a b", a=free_shape[0])
        elif len(free_shape) == 3:
            ap = ap.rearrange("p (a b c) -> p a b c", a=free_shape[0], b=free_shape[1])
        self.off += nb
        self.n += 1
        return Buf(ap, key if key is not None else ("sb", self.n))


class K:
    def __init__(self, stage, wseq=None):
        self.stage = stage
        self.prog = Prog()
        self.nc = bass.Bass("TRN2", target_bir_lowering=False)
        self.plan = wseq is None
        self.wseq = wseq if wseq is not None else []

    def E(self, eng, fn, outs, ins):
        reads = [k for b in ins if not b.ps for k in _keys(b.key)]
        writes = [k for b in outs for k in _keys(b.key)] + [k for b in ins if b.ps for k in _keys(b.key)]
        self.prog.op(eng, fn, reads, writes)

    def raw(self, eng, name, outs, ins, **kw):
        kw2 = {k: (v.ap if isinstance(v, Buf) else v) for k, v in kw.items()}
        self.E(eng, lambda e: getattr(e, name)(**kw2), outs, ins)

    def mm(self, out, lhsT, rhs, start=True, stop=True, skip=False):
        o, l, r = out.ap, lhsT.ap, rhs.ap
        self.E("pe", lambda e: e.matmul(o, lhsT=l, rhs=r, start=start, stop=stop, skip_group_check=skip),
               [out], [lhsT, rhs])

    def tr(self, out, in_, ident):
        o, i, d = out.ap, in_.ap, ident.ap
        self.E("pe", lambda e: e.transpose(o, i, d), [out], [in_, ident])

    def act(self, out, in_, func, bias=None, scale=None, accum=None):
        o, i = out.ap, in_.ap
        kw = {}
        ins = [in_]
        outs = [out]
        if bias is not None:
            if isinstance(bias, Buf):
                kw["bias"] = bias.ap
                ins.append(bias)
            else:
                kw["bias"] = float(bias)
        if scale is not None:
            if isinstance(scale, Buf):
                kw["scale"] = scale.ap
                ins.append(scale)
            else:
                kw["scale"] = float(scale)
        if accum is not None:
            kw["accum_out"] = accum.ap
            outs.append(accum)
        self.E("act", lambda e: e.activation(out=o, in_=i, func=func, **kw), outs, ins)

    def tt(self, out, in0, in1, op, eng="dve"):
        o, a, b = out.ap, in0.ap, in1.ap
        self.E(eng, lambda e: e.tensor_tensor(out=o, in0=a, in1=b, op=op), [out], [in0, in1])

    def ts(self, out, in0, s1, op0, s2=None, op1=None, accum=None, eng="dve"):
        o, a = out.ap, in0.ap
        ins = [in0]
        outs = [out]
        if isinstance(s1, Buf):
            ins.append(s1)
            s1v = s1.ap
        else:
            s1v = float(s1)
        if isinstance(s2, Buf):
            ins.append(s2)
            s2v = s2.ap
        else:
            s2v = None if s2 is None else float(s2)
        kw = {}
        if op1 is not None:
            kw["op1"] = op1
        if accum is not None:
            kw["accum_out"] = accum.ap
            outs.append(accum)
        self.E(eng, lambda e: e.tensor_scalar(out=o, in0=a, scalar1=s1v, scalar2=s2v, op0=op0, **kw), outs, ins)

    def stt(self, out, in0, scalar, in1, op0, op1):
        o, a, b = out.ap, in0.ap, in1.ap
        ins = [in0, in1]
        if isinstance(scalar, Buf):
            ins.append(scalar)
            sv = scalar.ap
        else:
            sv = float(scalar)
        self.E("dve", lambda e: e.scalar_tensor_tensor(out=o, in0=a, scalar=sv, in1=b, op0=op0, op1=op1), [out], ins)

    def cp(self, out, in_, eng="dve"):
        o, i = out.ap, in_.ap
        if eng == "act":
            self.E("act", lambda e: e.activation(out=o, in_=i, func=AF.Copy), [out], [in_])
        else:
            self.E(eng, lambda e: e.tensor_copy(o, i), [out], [in_])

    def memset(self, out, val, eng="dve"):
        o = out.ap
        self.E(eng, lambda e: e.memset(o, float(val)), [out], [])

    def dma(self, q, out, in_, fresh=None):
        o, i = out.ap, in_.ap
        if fresh is None:
            fresh = (q == "pool")
        self.prog.dma(q, lambda e: e.dma_start(out=o, in_=i), _keys(in_.key), _keys(out.key), fresh=fresh)

    def build(self):
        nc = self.nc
        dr = lambda name, shape, kind="ExternalInput": Buf(nc.dram_tensor(name, list(shape), F32, kind=kind).ap(), None)
        I = self.inp = {}
        I["x_seq"] = dr("x_seq", [SEQ, D])
        I["x_own"] = dr("x_own", [SEQ // 2, D])
        I["x_smp"] = dr("x_smp", [DEC_T, D])
        I["s0"] = dr("s0", [4, DK, DV])
        I["ck"] = dr("ck", [PAST, 256])
        I["cv"] = dr("cv", [PAST, 256])
        I["cki"] = dr("cki", [PAST, 64])
        I["wa"] = dr("wa", [128, WA_TOT])
        I["we"] = dr("we", [128, WE_TOT])
        I["wup"] = dr("wup", [17, 512])
        I["gn"] = dr("gn", [2, 256])
        I["ln"] = dr("ln", [4, D])
        I["wr"] = dr("wr", [128, 8 * 20])
        I["br"] = dr("br", [1, 20])
        I["sel"] = dr("sel", [16, 16 * 128])
        I["cst"] = dr("cst", [128, C_TOT])
        I["rope_seq"] = dr("rope_seq", [SEQ, 192])
        I["rope_own"] = dr("rope_own", [SEQ // 2, 192])
        O = self.out = {}
        O["y_own"] = dr("y_own", [SEQ // 2, D], "ExternalOutput")
        O["y_smp"] = dr("y_smp", [DEC_T, D], "ExternalOutput")
        O["st_p"] = dr("st_p", [4, DK, DV], "ExternalOutput")
        O["k_p"] = dr("k_p", [SEQ, 256], "ExternalOutput")
        O["v_p"] = dr("v_p", [SEQ, 256], "ExternalOutput")
        O["ki_p"] = dr("ki_p", [SEQ, 64], "ExternalOutput")
        O["st_s"] = dr("st_s", [4, DK, DV], "ExternalOutput")
        O["k_s"] = dr("k_s", [DEC_T, 256], "ExternalOutput")
        O["v_s"] = dr("v_s", [DEC_T, 256], "ExternalOutput")
        O["ki_s"] = dr("ki_s", [DEC_T, 64], "ExternalOutput")
        self.hscr = Buf(nc.dram_tensor("hscr", [17 * 128, D], F32, kind="Internal").ap(), "hscr")
        self.wa16 = Buf(nc.dram_tensor("wa16", [128, WA_TOT], BF16, kind="Internal").ap(), "wa16")
        self.we16 = Buf(nc.dram_tensor("we16", [128, WE_TOT], BF16, kind="Internal").ap(), "we16")

        SB_BYTES = 212832
        with contextlib.ExitStack() as es:
            big = es.enter_context(nc.sbuf_tensor("big", [128, SB_BYTES // 4], F32))
            self.ar = Arena(big, SB_BYTES)
            self.psb = []
            for k in range(8):
                t = es.enter_context(nc.psum_tensor(f"ps{k}", [128, 512], F32))
                self.psb.append(Buf(t[:], ("ps", k), True))
            sems = {e: es.enter_context(nc.semaphore(f"s_{e}")) for e in Prog.ENGS}
            dsems = [es.enter_context(nc.semaphore(f"d_{k}")) for k in range(self.prog.n_dma + self.prog.n_fresh)]

            self.program()
            if self.plan:
                return None

            block = es.enter_context(nc.Block())
            prog = self.prog

            def make(en):
                def body(e):
                    for waits, fn, inc in prog.ops[en]:
                        for sk, v in waits:
                            if isinstance(sk, tuple):
                                e.wait_ge(dsems[sk[1]], 16 * v)
                            else:
                                e.wait_ge(sems[sk], v)
                        if fn is None:
                            continue
                        ins = fn(e)
                        if isinstance(inc, tuple):
                            ins.then_inc(dsems[inc[1]], 16)
                        else:
                            ins.then_inc(sems[inc], 1)
                    if en == "sp":
                        for k in range(len(prog.dma_cnt)):
                            if prog.dma_cnt[k]:
                                e.wait_ge(dsems[k], 16 * prog.dma_cnt[k])
                        for sk in ("pe", "act", "dve", "pool"):
                            if prog.cnt[sk]:
                                e.wait_ge(sems[sk], prog.cnt[sk])
                return body

            block.tensor(make("pe"))
            block.scalar(make("act"))
            block.vector(make("dve"))
            block.gpsimd(make("pool"))
            block.sync(make("sp"))
        return nc

    def ps(self, k, dtype=F32):
        b = self.psb[k]
        return b.bc(BF16) if dtype == BF16 else b

    def program(self):
        ar = self.ar
        I, O = self.inp, self.out
        A = ar.alloc
        CH = 8192
        for c in range((WA_TOT + CH - 1) // CH):
            a, b = c * CH, min(WA_TOT, (c + 1) * CH)
            self.dma("pool", self.wa16[:, a:b].rk(("wa16", c)), I["wa"][:, a:b].rk(("wa16", c - 1) if c else None))
        self.CH = CH
        self.we_chunks = list(range((WE_TOT + CH - 1) // CH)) if self.stage >= 4 else []
        cst = A([C_TOT], F32)
        self.dma("sp", cst, I["cst"])
        cs = lambda n: cst[:, _C[n][0]:_C[n][1]]
        self.identf = cs("ident")
        self.uneg = cs("uneg")
        self.cm = cs("cm")
        self.pow2 = cs("pow2")
        self.pm = cs("pm")
        self.admb = cs("admb")
        self.ident = A([128], BF16)
        self.cp(self.ident, self.identf, "dve")
        self.i4 = A([4, 128], BF16)
        for h in range(4):
            self.cp(self.i4[:, h, :], self.identf, "dve")
        self.wup = A([512], F32, parts=17)
        self.dma("sp", self.wup, I["wup"])
        self.gn = A([2, 256], F32)
        self.dma("sp", self.gn[:, 0, :], Buf(I["gn"].ap[0:1, :].partition_broadcast(128), None))
        self.dma("sp", self.gn[:, 1, :], Buf(I["gn"].ap[1:2, :].partition_broadcast(128), None))
        self.grx = A([128], F32, parts=17)
        self.memset(self.grx, 1.0)
        self.eps = A([1], F32)
        self.memset(self.eps, LN_EPS)
        self.cbt = A([32], F32)

        self.p2_mark = ar.off
        NKT = 32
        self.KT = A([2, NKT * 128], BF16, key="KTall")
        self.VX = A([NKT, 2, 130], BF16, key="VXall")
        self.KI = A([NKT * 128], BF16, key="KIall")
        self.memset(self.VX[:, :, :, 128:130].rk("VXones"), 1.0, "pool")
        self.kvp = {"KT": self.KT, "VX": self.VX, "KI": self.KI, "n": ""}
        self.S = A([4, 256], F32, key="S")
        self.memset(self.S, 0.0)
        self.wring = [A([8 * 512], BF16, key=("wring", i)) for i in range(4)]
        self.wcons = 0
        self.wissued = 0

        sl = []
        for s in range(2):
            d = {}
            if s == 1:
                self.sl1_off = ar.off
            d["xT"] = A([8, 128], BF16)
            d["En"] = A([512], F32)
            d["EpT"] = A([4, 128], F32)
            d["Ktm"] = A([512], BF16)
            d["KtT"] = A([4, 128], BF16)
            d["Vg"] = A([1024], BF16)
            d["Sbf"] = A([4, 256], BF16)
            sl.append(d)
        self.t12 = A([1024], F32)
        self.t1 = self.t12[:, 0:512]
        self.t2 = self.t12[:, 512:1024]
        for d in sl:
            d["L"] = self.t1
            d["e1"] = self.t2
        self.sl = sl
        off1 = self.sl1_off
        reg = self.ar.big[:, off1 // 4:(off1 + 12288) // 4].bitcast(BF16)
        k1 = MK(d_.key for d_ in (sl[1][n_] for n_ in ("xT", "En", "EpT", "Ktm", "KtT", "Vg", "Sbf")))
        self.kvs = {"KT": Buf(reg[:, 0:2304].rearrange("p (g t) -> p g t", g=2), k1),
                    "VX": Buf(reg[:, 2304:2304 + 2340].rearrange("p (t g d) -> p t g d", t=9, g=2), k1),
                    "KI": Buf(reg[:, 4672:4672 + 1152], k1), "n": "s"}
        self.xs = [A([1024], F32) for _ in range(2)]
        self.xb = A([1024], BF16)
        self.rt = [A([192], F32) for _ in range(2)]
        self.knew = A([256], F32)
        self.vnew = A([256], F32)
        self.kinew = A([64], F32)
        self.kb16 = A([256], BF16)
        self.kidup = A([128], BF16)
        self.xrr = 0
        if self.stage >= 2:
            self.xo2 = [A([1024], F32)] * 3
            self.xres = A([1024], F32)
            self.xoT2 = [A([8, 128], BF16) for _ in range(3)]
            self.sab = A([512], BF16)
            self.tbb = A([512], BF16)
            self.rto2 = [A([192], F32) for _ in range(2)]
            self.wi2 = [A([8], F32) for _ in range(2)]
            self.KtT_o = A([4, 128], BF16)
            self.Vg_o = A([1024], BF16)
            self.EpT_o = A([4, 128], F32)
            self.Sbf_o = A([4, 256], BF16)
            self.QtT = A([4, 128], BF16)
            self.AT = A([4, 128], BF16)
            self.sgg = A([1024], BF16)
            self.bufA = self.t12
            self.stg = [A([1024], BF16) for _ in range(3)]
            self.ogT2 = [A([8, 128], BF16) for _ in range(2)]
            self.qT2 = [A([8, 128], BF16) for _ in range(2)]
            self.qir = A([512], BF16)
            self.qiT = A([4, 128], BF16)
            self.odT = A([8, 128], BF16)
            self.mT = A([8, 128], BF16)
            self.idx = A([4096], F32)
            self.mb = A([4096], BF16)
            self.rbuf = [A([512], F32) for _ in range(2)]
            self.PTb = [A([4, 128], BF16) for _ in range(3)]
            self.sm = A([128], F32)
            self.cstage = self.mb[:, 0:2048].rr("p (t c) -> p t c", t=8)
            self.kistage = self.mb[:, 2048:3072].rr("p (t c) -> p t c", t=8)
        print("phase-1 SBUF bytes/partition:", ar.off)

        self.prompt()
        if self.stage < 3:
            self.dma("sp", O["st_p"].rr("h k v -> k h v"), self.S)
        if self.stage >= 4:
            self.phase2()

    def load_xT(self, src_rows, nt, xT, xs=None, trb=2, load=True):
        if xs is None:
            xs = self.xs[self.xrr % 2]
            self.xrr += 1
        if load:
            self.dma("sp", xs[0:nt, :], src_rows)
        self.cp(self.xb[0:nt, :], xs[0:nt, :], "dve")
        pt = self.ps(trb, BF16)
        for k in range(8):
            self.tr(pt[:, k * nt:(k + 1) * nt], self.xb[0:nt, k * 128:(k + 1) * 128], self.ident[0:nt, 0:nt])
        self.cp(xT[:, :, 0:nt], pt[:, 0:8 * nt].rr("p (k t) -> p k t", k=8), "dve")
        return xs

    def rope(self, out, src, tab, nt, H, h):
        cosb, sinb = tab
        x4 = src.rr("p (a b c) -> p a b c", a=H, b=2)
        t1 = self.t1[0:nt, 0:H * 2 * h].rr("p (a b c) -> p a b c", a=H, b=2)
        t2 = self.t2[0:nt, 0:H * 2 * h].rr("p (a b c) -> p a b c", a=H, b=2)
        o4 = out.rr("p (a b c) -> p a b c", a=H, b=2)
        c4 = cosb.uq(1).uq(1).bt([nt, H, 2, h])
        s3 = sinb.uq(1).bt([nt, H, h])
        self.tt(t1, x4, c4, ALU.mult)
        self.tt(t2[:, :, 0, :], x4[:, :, 1, :], s3, ALU.mult)
        self.tt(t2[:, :, 1, :], x4[:, :, 0, :], s3, ALU.mult)
        self.tt(o4[:, :, 0, :], t1[:, :, 0, :], t2[:, :, 0, :], ALU.subtract)
        self.tt(o4[:, :, 1, :], t1[:, :, 1, :], t2[:, :, 1, :], ALU.add)

    def wget(self, name):
        if self.plan:
            self.wseq.append(name)
        assert self.wseq[self.wcons] == name, (self.wseq[self.wcons], name)
        while self.wissued < min(len(self.wseq), self.wcons + len(self.wring)):
            n = self.wseq[self.wissued]
            slot = self.wring[self.wissued % len(self.wring)]
            c = _BLK_COLS[n]
            off = _BLK_OFF[n]
            chs = MK(("wa16", q) for q in range(off // self.CH, (off + 8 * c - 1) // self.CH + 1))
            self.dma("sp", slot[:, 0:8 * c], self.wa16[:, off:off + 8 * c].rk(chs))
            self.wissued += 1
        slot = self.wring[self.wcons % len(self.wring)]
        c = _BLK_COLS[name]
        self.wcons += 1
        return slot[:, 0:8 * c].rr("p (k c) -> p k c", k=8)

    def sh_sm(self, s, nt, w, kt, ki_out, kv=None):
        kv = kv or self.kvp
        d = self.sl[s]
        xT = d["xT"]
        rt = self.rt[s]
        pg = self.ps(4)
        pt = self.ps(3, BF16)
        for k in range(8):
            self.mm(pg[0:16, 0:nt], w[:, k, 0:16], xT[:, k, 0:nt], start=(k == 0), stop=(k == 7))
        self.cp(self.grx[0:16, 0:nt], pg[0:16, 0:nt], "dve")
        pz = self.ps(s)
        for k in range(8):
            self.mm(pz[0:nt, 0:64], xT[:, k, 0:nt], w[:, k, 16:80], start=(k == 0), stop=(k == 7))
        self.rope(self.kinew[0:nt, :], pz[0:nt, 0:64], (rt[0:nt, 128:160], rt[0:nt, 160:192]), nt, 1, 32)
        self.dma("sp", ki_out, self.kinew[0:nt, :])
        self.cp(self.kidup[0:nt, 0:64], self.kinew[0:nt, :], "pool")
        self.cp(self.kidup[0:nt, 64:128], self.kinew[0:nt, :], "pool")
        self.tr(pt[:, 0:nt], self.kidup[0:nt, :], self.ident[0:nt, 0:nt])
        self.cp(kv["KI"][:, kt * 128:kt * 128 + nt].rk((kv["n"] + "KI", kt)), pt[:, 0:nt], "dve")
        self.mm(pg[0:nt, :], self.grx[0:17, 0:nt], self.wup[0:17, :])
        self.act(d["e1"][0:nt, :], pg[0:nt, :], AF.Exp, scale=-1.0)
        self.act(d["L"][0:nt, :], d["e1"][0:nt, :], AF.Ln, bias=1.0)
        self.mm(pg[0:nt, :], self.uneg[0:nt, 0:nt], d["L"][0:nt, :])
        self.act(d["En"][0:nt, :], pg[0:nt, :], AF.Exp, scale=-1.0)
        for h in range(4):
            self.mm(pg[:, h * nt:(h + 1) * nt], d["L"][0:nt, h * 128:(h + 1) * 128], self.uneg[0:nt, 0:nt])
        self.act(d["EpT"][:, :, 0:nt], pg[:, 0:4 * nt].rr("p (h t) -> p h t", h=4), AF.Exp)

    def sh_gk(self, s, nt, w):
        d = self.sl[s]
        xT = d["xT"]
        pz = self.ps(s)
        pt = self.ps(3, BF16)
        for k in range(8):
            self.mm(pz[0:nt, :], xT[:, k, 0:nt], w[:, k, :], start=(k == 0), stop=(k == 7))
        self.tt(d["Ktm"][0:nt, :], pz[0:nt, :], d["En"][0:nt, :], ALU.mult)
        for h in range(4):
            self.tr(pt[:, h * nt:(h + 1) * nt], d["Ktm"][0:nt, h * 128:(h + 1) * 128], self.ident[0:nt, 0:nt])
        self.cp(d["KtT"][:, :, 0:nt], pt[:, 0:4 * nt].rr("p (h t) -> p h t", h=4), "dve")

    def sh_gv(self, s, nt, w, c):
        d = self.sl[s]
        pz = self.ps(s)
        for k in range(8):
            self.mm(pz[0:nt, :], d["xT"][:, k, 0:nt], w[:, k, :], start=(k == 0), stop=(k == 7))
        self.cp(d["Vg"][0:nt, c * 512:(c + 1) * 512], pz[0:nt, :], "dve")

    def sh_dkv(self, s, nt, w, kt, k_out, v_out, kv=None):
        kv = kv or self.kvp
        d = self.sl[s]
        rt = self.rt[s]
        pz = self.ps(s)
        pt = self.ps(3, BF16)
        for k in range(8):
            self.mm(pz[0:nt, :], d["xT"][:, k, 0:nt], w[:, k, :], start=(k == 0), stop=(k == 7))
        self.rope(self.knew[0:nt, :], pz[0:nt, 0:256], (rt[0:nt, 0:64], rt[0:nt, 64:128]), nt, 2, 64)
        self.cp(self.vnew[0:nt, :], pz[0:nt, 256:512], "dve")
        self.cp(kv["VX"][0:nt, kt, :, 0:128].rk((kv["n"] + "VX", kt)), pz[0:nt, 256:512].rr("p (g d) -> p g d", g=2), "dve")
        self.dma("sp", k_out, self.knew[0:nt, :])
        self.dma("sp", v_out, self.vnew[0:nt, :])
        self.cp(self.kb16[0:nt, :], self.knew[0:nt, :], "pool")
        for g in range(2):
            self.tr(pt[:, g * nt:(g + 1) * nt], self.kb16[0:nt, g * 128:(g + 1) * 128], self.ident[0:nt, 0:nt])
        self.cp(kv["KT"][:, :, kt * 128:kt * 128 + nt].rk((kv["n"] + "KT", kt)), pt[:, 0:2 * nt].rr("p (g t) -> p g t", g=2), "dve")

    def sh_state(self, s, nt):
        d = self.sl[s]
        self.cp(d["Sbf"], self.S, "pool")
        for hp in range(2):
            pst = self.ps(hp)
            for hh in range(2):
                h = 2 * hp + hh
                self.mm(pst[:, hh * 256:(hh + 1) * 256], d["Ktm"][0:nt, h * 128:(h + 1) * 128],
                        d["Vg"][0:nt, h * 256:(h + 1) * 256])
            for hh in range(2):
                h = 2 * hp + hh
                el = d["EpT"][:, h, nt - 1:nt]
                self.ts(self.S[:, h, :], self.S[:, h, :], el, ALU.mult)
                self.stt(self.S[:, h, :], pst[:, hh * 256:(hh + 1) * 256], el, self.S[:, h, :], ALU.mult, ALU.add)

    def prefetch_x(self, j):
        I = self.inp
        for s in range(2):
            r0 = (2 * j + s) * 128
            self.dma("sp", self.xs[s][0:128, :], I["x_seq"][r0:r0 + 128, :])
            self.dma("sp", self.rt[s][0:128, :], I["rope_seq"][r0:r0 + 128, :])
        if self.stage >= 2:
            b = j % 2
            self.dma("sp", self.xo2[j % 3][0:128, :], I["x_own"][j * 128:(j + 1) * 128, :])
            self.dma("sp", self.rto2[b][0:128, :], I["rope_own"][j * 128:(j + 1) * 128, :])

    def g_shared(self, j):
        I, O = self.inp, self.out
        tiles = []
        for s in range(2):
            i = 2 * j + s
            r0 = i * 128
            self.load_xT(None, 128, self.sl[s]["xT"], self.xs[s], trb=3, load=False)
            tiles.append((s, i, r0))
            yield 6.0
        b = j % 2
        if self.stage >= 2:
            self.load_xT(None, 128, self.xoT2[j % 3], self.xo2[j % 3], trb=3, load=False)
            self.flag_xo = j
            yield 6.0
        w = self.wget("sm")
        for s, i, r0 in tiles:
            self.sh_sm(s, 128, w, i, O["ki_p"][r0:r0 + 128, :])
        if self.stage >= 2:
            self.own_wi(w, self.xoT2[j % 3], 128, self.wi2[b])
        self.flag_ki = j
        yield 14.0
        w = self.wget("gk")
        for s, i, r0 in tiles:
            self.sh_gk(s, 128, w)
        yield 10.0
        for c in range(2):
            w = self.wget(f"gv{c}")
            for s, i, r0 in tiles:
                self.sh_gv(s, 128, w, c)
            yield 8.0
        w = self.wget("dkv")
        for s, i, r0 in tiles:
            self.sh_dkv(s, 128, w, i, O["k_p"][r0:r0 + 128, :], O["v_p"][r0:r0 + 128, :])
        yield 14.0
        for s, i, r0 in tiles:
            self.sh_state(s, 128)
            yield 6.0

    def run(self, gen):
        for _ in gen:
            pass

    def interleave(self, gens, until=None):
        until = list(range(len(gens))) if until is None else until
        while not all(gens[k][2] for k in until):
            act = [k for k in range(len(gens)) if not gens[k][2]]
            k = min(act, key=lambda q: gens[q][1])
            try:
                c = next(gens[k][0])
                if c == 0.0:
                    others = [gens[q][1] for q in act if q != k]
                    gens[k][1] = (max(others) if others else gens[k][1]) + 1e-3
                else:
                    gens[k][1] += (c or 0.0) * (gens[k][3] if len(gens[k]) > 3 else 1.0)
            except StopIteration:
                gens[k][2] = True

    def pair_args(self, j):
        b = j % 2
        return dict(nt=128, L=256 * (j + 1), xoT=self.xoT2[j % 3], xo=self.xo2[j % 3], rto=self.rto2[b], KtT=self.KtT_o,
                    Vg=self.Vg_o, EpT=self.EpT_o, Sbf=self.Sbf_o, admb=self.admb, slot=j, wi=self.wi2[b], par=b)

    def g_pre1(self, j):
        yield from self.g_shared(j)
        if self.stage < 2:
            return
        m0, m1 = self.pm[:, 0:1], self.pm[:, 1:2]
        for name, dst in (("KtT", self.KtT_o), ("Vg", self.Vg_o), ("EpT", self.EpT_o), ("Sbf", self.Sbf_o)):
            self.ts(dst, self.sl[0][name], m0, ALU.mult)
            self.stt(dst, self.sl[1][name], m1, dst, ALU.mult, ALU.add)
        yield 6.0
        yield from self.g_front_gla(**self.pair_args(j))

    def g_pre2(self, j, wait=None):
        if self.stage < 2:
            return
        while self.flag_xo < j:
            yield 0.0
        w2 = (lambda: self.flag_ki >= j and (wait is None or wait()))
        yield from self.g_front_q(wait=w2, **self.pair_args(j))

    def prompt(self):
        self.flag_xo = -1
        self.flag_ki = -1
        self.prefetch_x(0)
        self.bisect_done = True
        self.interleave([[self.g_pre1(0), 0.0, False], [self.g_pre2(0), 0.0, False]])
        back = None
        for j in range(NPAIR):
            if j + 1 < NPAIR:
                self.prefetch_x(j + 1)
            if self.stage < 2:
                if j + 1 < NPAIR:
                    self.run(self.g_pre1(j + 1))
                continue
            if j >= 1 and self.we_chunks:
                c = self.we_chunks.pop(0)
                a_, b_ = c * self.CH, min(WE_TOT, (c + 1) * self.CH)
                self.dma("pool", self.we16[:, a_:b_].rk(("we16", c)), self.inp["we"][:, a_:b_])
            args = self.pair_args(j)
            self.bisect_done = False
            self.attn_done = False
            self.back_done = False
            if j + 1 < NPAIR:
                B1 = [self.g_pre1(j + 1), 0.0, False]
                B2 = [self.g_pre2(j + 1, wait=lambda: self.bisect_done and self.attn_done), 0.0, False]
            elif self.stage >= 3:
                self.dma("sp", self.out["st_p"].rr("h k v -> k h v"), self.S)
                self.flag_sx = False
                self.flag_sk = False
                B1 = [self.g_sample_pre1(), 0.0, False]
                B2 = [self.g_sample_pre2(lambda: self.bisect_done and self.attn_done), 0.0, False]
            else:
                B1 = [iter(()), 0.0, False]
                B2 = [iter(()), 0.0, False]
            s1 = [[self.g_bisect(**args), 0.0, False], [back if back is not None else iter(()), 0.0, False, 0.7], B1, B2]
            B1.append(BSCALE)
            B2.append(BSCALE)
            self.interleave(s1, until=[0, 1])
            B1[3] = 1.3
            B2[3] = 1.3
            self.back_done = True
            B1[1] = 0.0
            B2[1] = 0.0
            s2 = [[self.g_attn(**args), 0.0, False], B1, B2]
            self.interleave(s2)
            back = self.g_back(**args)
        if self.stage >= 3:
            sargs = self.sample_args()
            self.interleave([[self.g_bisect(**sargs), 0.0, False], [back, 0.0, False]])
            self.run(self.g_attn(**sargs))
            self.run(self.g_back(**sargs))
        elif back is not None:
            self.run(back)

    def own_wi(self, wsm, xoT, nt, wi):
        pz = self.ps(1)
        for k in range(8):
            self.mm(pz[0:nt, 64:72], xoT[:, k, 0:nt], wsm[:, k, 80:88], start=(k == 0), stop=(k == 7))
        self.ts(wi[0:nt, 0:8], pz[0:nt, 64:72], IDX_W_SCALE, ALU.mult)

    def transposes(self, dstT, src_tm, nt, nblk, bank=2):
        pt = self.ps(bank, BF16)
        for b in range(nblk):
            self.tr(pt[:, b * nt:(b + 1) * nt], src_tm[0:nt, b * 128:(b + 1) * 128], self.ident[0:nt, 0:nt])
        self.cp(dstT[:, 0:nblk, 0:nt], pt[:, 0:nblk * nt].rr("p (b t) -> p b t", b=nblk), "dve")

    def layernorm_stats(self, src, nt, ngrp, width, mv, st, rs):
        for g in range(ngrp):
            self.raw("dve", "bn_stats", [st], [src], out=st[0:nt, g * 6:(g + 1) * 6], in_=src[0:nt, g * width:(g + 1) * width])
        self.raw("dve", "bn_aggr", [mv], [st], out=mv[0:nt, 0:2], in_=st[0:nt, 0:6 * ngrp])
        self.ts(rs[0:nt, 0:1], mv[0:nt, 1:2], LN_EPS, ALU.add, eng="pool")
        self.tt(rs[0:nt, 0:1], rs[0:nt, 0:1], self.cbias(-0.5)[0:nt, :], ALU.pow, eng="pool")

    def g_front_gla(self, nt, L, xoT, xo, rto, KtT, Vg, EpT, Sbf, admb, slot, wi, par=0, wait=None, kv=None):
        kv = kv or self.kvp
        ogT = self.ogT2[par]
        I, O = self.inp, self.out
        sm = self.sm
        w = self.wget("gq")
        pq = self.ps(3)
        for h in range(4):
            for k in range(8):
                self.mm(pq[:, h * nt:(h + 1) * nt], w[:, k, h * 128:(h + 1) * 128], xoT[:, k, 0:nt],
                        start=(k == 0), stop=(k == 7))
        self.stt(self.QtT[:, :, 0:nt], pq[:, 0:4 * nt].rr("p (h t) -> p h t", h=4), DK ** -0.5,
                 EpT[:, :, 0:nt], ALU.mult, ALU.mult)
        yield 4.0
        pa = self.ps(4)
        for h in range(4):
            self.mm(pa[0:nt, h * nt:(h + 1) * nt], KtT[:, h, 0:nt], self.QtT[:, h, 0:nt])
        self.tt(self.AT[0:nt, :, 0:nt], pa[0:nt, 0:4 * nt].rr("p (h t) -> p h t", h=4),
                self.cm[0:nt, 0:nt].uq(1).bt([nt, 4, nt]), ALU.mult)
        for c in range(2):
            w = self.wget(f"gg{c}")
            pz = self.ps(c)
            for k in range(8):
                self.mm(pz[0:nt, :], xoT[:, k, 0:nt], w[:, k, :], start=(k == 0), stop=(k == 7))
            self.act(self.sgg[0:nt, c * 512:(c + 1) * 512], pz[0:nt, :], AF.Silu)
            yield 3.0
        on = self.bufA
        for hp in range(2):
            po = self.ps(hp)
            for hh in range(2):
                h = 2 * hp + hh
                self.mm(po[0:nt, hh * 256:(hh + 1) * 256], self.AT[0:nt, h, 0:nt], Vg[0:nt, h * 256:(h + 1) * 256],
                        start=True, stop=False)
                self.mm(po[0:nt, hh * 256:(hh + 1) * 256], self.QtT[:, h, 0:nt], Sbf[:, h, :],
                        start=False, stop=True)
            for hh in range(2):
                h = 2 * hp + hh
                st, mv, rs = sm[:, 16:22], sm[:, 22:24], sm[:, 24:25]
                self.layernorm_stats(po[:, hh * 256:(hh + 1) * 256], nt, 1, 256, mv, st, rs)
                self.ts(on[0:nt, h * 256:(h + 1) * 256], po[0:nt, hh * 256:(hh + 1) * 256], mv[0:nt, 0:1], ALU.subtract,
                        rs[0:nt, 0:1], ALU.mult)
        on3 = on[0:nt, :].rr("p (h v) -> p h v", h=4)
        self.tt(on3, on3, self.gn[0:nt, 0, :].uq(1).bt([nt, 4, 256]), ALU.mult)
        self.tt(on3, on3, self.gn[0:nt, 1, :].uq(1).bt([nt, 4, 256]), ALU.add)
        og = self.stg[1]
        self.tt(og[0:nt, :], on[0:nt, :], self.sgg[0:nt, :], ALU.mult)
        self.transposes(ogT, og, nt, 8, bank=3)
        yield 18.0

    def g_front_q(self, nt, L, xoT, xo, rto, KtT, Vg, EpT, Sbf, admb, slot, wi, par=0, wait=None, kv=None):
        kv = kv or self.kvp
        sm = self.sm
        qT = self.qT2[par]
        qr = self.stg[2]
        for c in range(2):
            w = self.wget(f"dq{c}")
            pz = self.ps(c)
            for k in range(8):
                self.mm(pz[0:nt, :], xoT[:, k, 0:nt], w[:, k, :], start=(k == 0), stop=(k == 7))
            self.rope(qr[0:nt, c * 512:(c + 1) * 512], pz[0:nt, :], (rto[0:nt, 0:64], rto[0:nt, 64:128]), nt, 4, 64)
            yield 5.0
        self.transposes(qT, qr, nt, 8, bank=3)
        yield 3.0
        w = self.wget("iq")
        pz = self.ps(0)
        for k in range(8):
            self.mm(pz[0:nt, :], xoT[:, k, 0:nt], w[:, k, :], start=(k == 0), stop=(k == 7))
        self.rope(self.qir[0:nt, :], pz[0:nt, :], (rto[0:nt, 128:160], rto[0:nt, 160:192]), nt, 8, 32)
        self.transposes(self.qiT, self.qir, nt, 4, bank=3)
        yield 6.0
        while wait is not None and not wait():
            yield 0.0
        idx = self.idx
        nkb = (L + 511) // 512
        ri = 0
        for kb in range(nkb):
            n = min(512, L - kb * 512)
            kts = MK((kv["n"] + "KI", t) for t in range(kb * 4, (kb * 512 + n + 127) // 128))
            blk = idx[0:nt, kb * 512:kb * 512 + n]
            for hp in range(4):
                pab = (self.ps(0), self.ps(1)) if hp % 2 == 0 else (self.ps(3), self.ps(4))
                for hh in range(2):
                    lo, hi = (0, 64) if hh == 0 else (64, 128)
                    self.mm(pab[hh][0:nt, 0:n], self.qiT[lo:hi, hp, 0:nt],
                            kv["KI"][lo:hi, kb * 512:kb * 512 + n].rk(kts))
                for hh in range(2):
                    h = 2 * hp + hh
                    r = self.rbuf[ri % 2]
                    ri += 1
                    self.act(r[0:nt, 0:n], pab[hh][0:nt, 0:n], AF.Relu)
                    if h == 0:
                        self.ts(blk, r[0:nt, 0:n], wi[0:nt, 0:1], ALU.mult)
                    else:
                        self.stt(blk, r[0:nt, 0:n], wi[0:nt, h:h + 1], blk, ALU.mult, ALU.add)
                yield 1.3 * n / 512

    def g_bisect(self, nt, L, xoT, xo, rto, KtT, Vg, EpT, Sbf, admb, slot, wi, par=0, wait=None, kv=None):
        kv = kv or self.kvp
        sm = self.sm
        idx = self.idx
        amax, lo_, mid, u = sm[:, 32:33], sm[:, 33:34], sm[:, 34:35], sm[:, 35:36]
        wk = sm[:, 40:40 + NIT + 1]
        sa = sm[:, 64:64 + NIT]
        if L > TOPK:
            self.raw("dve", "tensor_reduce", [amax], [idx], out=amax[0:nt, :], in_=idx[0:nt, 0:L], axis=AX.X,
                     op=ALU.max, apply_absolute_value=True)
        if admb is not None:
            self.tt(idx[0:nt, L - 256:L], idx[0:nt, L - 256:L], admb[0:nt, :], ALU.add)
        if L > TOPK:
            c0 = float(L - 2 * TOPK) + 0.5
            self.ts(wk[0:nt, :], self.pow2[0:nt, 0:NIT + 1], amax[0:nt, :], ALU.mult, 2.0002, ALU.mult)
            self.tt(mid[0:nt, :], wk[0:nt, 0:1], amax[0:nt, :], ALU.subtract)
            yield 3.0
            for k in range(NIT):
                self.act(self.mb[0:nt, 0:L], idx[0:nt, 0:L], AF.Sign, bias=mid[0:nt, :], scale=-1.0,
                         accum=sa[0:nt, k:k + 1])
                self.act(u[0:nt, :], sa[0:nt, k:k + 1], AF.Sign, bias=self.cbias(c0)[0:nt, :], scale=-1.0)
                self.act(mid[0:nt, :], u[0:nt, :], AF.Identity, bias=mid[0:nt, :], scale=wk[0:nt, k + 1:k + 2])
                yield 1.1 + L / 1200.0
            self.tt(lo_[0:nt, :], mid[0:nt, :], wk[0:nt, NIT:NIT + 1], ALU.subtract)
        else:
            self.memset(lo_[0:nt, :], -1e29)
        self.ts(self.mb[0:nt, 0:L], idx[0:nt, 0:L], lo_[0:nt, :], ALU.is_lt, NEG, ALU.mult)
        self.bisect_done = True
        yield 1.0 + L / 1900.0

    def cbias(self, val):
        if not hasattr(self, "_cb"):
            self._cb = {}
            self._cbt = self.cbt
            self._cbn = 0
        if val not in self._cb:
            t = self._cbt[:, self._cbn:self._cbn + 1]
            self._cbn += 1
            assert self._cbn <= 32
            self.memset(t, val, "pool")
            self._cb[val] = t
        return self._cb[val]

    def g_attn(self, nt, L, xoT, xo, rto, KtT, Vg, EpT, Sbf, admb, slot, wi, par=0, wait=None, kv=None):
        kv = kv or self.kvp
        qT_ = self.qT2[par]
        sm = self.sm
        od = self.stg[0]
        nkt = (L + 127) // 128
        seq = [(g, kt) for g in range(2) for kt in range(nkt)]

        def scores(n):
            g, kt = seq[n]
            nk = min(128, L - kt * 128)
            pS = self.ps(2 if n % 2 == 0 else 5)
            ST = pS[0:nk, 0:4 * nt].rr("p (h t) -> p h t", h=4)
            self.mm(ST, kv["KT"][:, g, kt * 128:kt * 128 + nk].rk((kv["n"] + "KT", kt)), qT_[:, 4 * g:4 * g + 4, 0:nt],
                    start=True, stop=False)
            self.mm(ST, self.mb[0:nt, kt * 128:kt * 128 + nk], self.i4[0:nt, :, 0:nt], start=False, stop=True)
            return ST, nk

        cur = scores(0)
        for n, (g, kt) in enumerate(seq):
            ST, nk = cur
            if n + 1 < len(seq):
                cur = scores(n + 1)
            PT = self.PTb[n % 3]
            self.act(PT[0:nk, :, 0:nt], ST, AF.Exp, scale=ATT_SCALE)
            for hh in range(4):
                ob = self.ps(6)[0:nt, hh * 130:hh * 130 + 129] if hh < 3 else self.ps(7)[0:nt, 0:129]
                self.mm(ob, PT[0:nk, hh, 0:nt], kv["VX"][0:nk, kt, g, 0:129].rk(MK(((kv["n"] + "VX", kt), kv["n"] + "VXones"))),
                        start=(kt == 0 and hh in (0, 3)), stop=(kt == nkt - 1), skip=True)
            if kt == nkt - 1:
                for hh in range(4):
                    ob = self.ps(6)[0:nt, hh * 130:hh * 130 + 129] if hh < 3 else self.ps(7)[0:nt, 0:129]
                    rd = sm[:, 36 + hh:37 + hh]
                    self.raw("dve", "reciprocal", [rd], [ob], out=rd[0:nt, :], in_=ob[:, 128:129])
                    hq = 4 * g + hh
                    self.ts(od[0:nt, hq * 128:(hq + 1) * 128], ob[:, 0:128], rd[0:nt, :], ALU.mult)
            if n == len(seq) - 1:
                self.attn_done = True
            yield 1.7

    def g_back(self, nt, L, xoT, xo, rto, KtT, Vg, EpT, Sbf, admb, slot, wi, par=0, wait=None, kv=None):
        kv = kv or self.kvp
        ogT_ = self.ogT2[par]
        if slot < 16:
            xo = self.xres
            self.dma("sp", xo[0:nt, :], self.inp["x_own"][slot * 128:(slot + 1) * 128, :])
        I, O = self.inp, self.out
        sm = self.sm
        od = self.stg[0]
        self.transposes(self.odT, od, nt, 8, bank=6)
        yield 3.0
        m = self.stg[0]
        sa, tb = self.sab, self.tbb
        for c in range(2):
            w = self.wget(f"ga{c}")
            pz = self.ps(2)
            for k in range(8):
                self.mm(pz[0:nt, :], xoT[:, k, 0:nt], w[:, k, :], start=(k == 0), stop=(k == 7))
            self.act(sa[0:nt, :], pz[0:nt, :], AF.Sigmoid)
            w = self.wget(f"wg{c}")
            pz = self.ps(5)
            for k in range(8):
                self.mm(pz[0:nt, :], ogT_[:, k, 0:nt], w[:, k, :], start=(k == 0), stop=(k == 7))
            self.tt(tb[0:nt, :], pz[0:nt, :], sa[0:nt, :], ALU.mult)
            yield 4.0
            w = self.wget(f"gb{c}")
            pz = self.ps(2)
            for k in range(8):
                self.mm(pz[0:nt, :], xoT[:, k, 0:nt], w[:, k, :], start=(k == 0), stop=(k == 7))
            self.act(sa[0:nt, :], pz[0:nt, :], AF.Sigmoid)
            w = self.wget(f"wd{c}")
            pz = self.ps(5)
            for k in range(8):
                self.mm(pz[0:nt, :], self.odT[:, k, 0:nt], w[:, k, :], start=(k == 0), stop=(k == 7))
            self.tt(sa[0:nt, :], pz[0:nt, :], sa[0:nt, :], ALU.mult)
            self.tt(m[0:nt, c * 512:(c + 1) * 512], sa[0:nt, :], tb[0:nt, :], ALU.add)
            yield 4.0
        self.transposes(self.mT, m, nt, 8, bank=6)
        yield 3.0
        v1 = xo
        for c in range(2):
            w = self.wget(f"wo{c}")
            pz = self.ps(2 if c == 0 else 5)
            for k in range(8):
                self.mm(pz[0:nt, :], self.mT[:, k, 0:nt], w[:, k, :], start=(k == 0), stop=(k == 7))
            self.stt(v1[0:nt, c * 512:(c + 1) * 512], xo[0:nt, c * 512:(c + 1) * 512], ALPHA, pz[0:nt, :],
                     ALU.mult, ALU.add)
        yield 4.0
        st, mv, rs = sm[:, 84:96], sm[:, 96:98], sm[:, 98:99]
        self.layernorm_stats(v1, nt, 2, 512, mv, st, rs)
        self.ts(v1[0:nt, :], v1[0:nt, :], mv[0:nt, 0:1], ALU.subtract, rs[0:nt, 0:1], ALU.mult)
        self.dma("sp", self.hscr[slot * 128:slot * 128 + nt, :].rk(("hscr", slot)), v1[0:nt, :])
        if self.stage == 2 or self.stage == 3:
            dst = O["y_own"][slot * 128:(slot + 1) * 128, :] if slot < 16 else O["y_smp"]
            self.dma("sp", dst, v1[0:nt, :])

    def sample_args(self):
        d = self.sl[0]
        return dict(nt=DEC_T, L=PAST + DEC_T, xoT=d["xT"], xo=self.xo2[0], rto=self.rt[0], KtT=d["KtT"], Vg=d["Vg"],
                    EpT=d["EpT"], Sbf=d["Sbf"], admb=None, slot=16, wi=self.wi2[0], par=0, kv=self.kvs)

    def g_sample_pre1(self):
        I, O = self.inp, self.out
        nt = DEC_T
        kv = self.kvs
        pt = self.ps(3, BF16)
        cstage = self.xs[0].bc(BF16)[:, 0:2048].rr("p (t c) -> p t c", t=8)
        kistage = self.xs[1].bc(BF16)[:, 0:1024].rr("p (t c) -> p t c", t=8)
        self.memset(kv["VX"][:, :, :, 128:130].rk("sVXones"), 1.0, "pool")
        self.dma("pool", cstage, I["ck"].rr("(t p) c -> p t c", p=128))
        self.dma("pool", kistage[:, :, 0:64], I["cki"].rr("(t p) c -> p t c", p=128))
        self.dma("pool", kistage[:, :, 64:128], I["cki"].rr("(t p) c -> p t c", p=128))
        for g in range(2):
            self.dma("pool", kv["VX"][:, 0:8, g, 0:128].rk(MK(("sVX", t) for t in range(8))),
                     I["cv"][:, g * 128:(g + 1) * 128].rr("(t p) d -> p t d", p=128))
        self.dma("sp", self.S, I["s0"].rr("h k v -> k h v"))
        d = self.sl[0]
        self.load_xT(I["x_smp"], nt, d["xT"], self.xo2[0], trb=3)
        self.dma("sp", self.rt[0][0:nt, :], I["rope_seq"][PAST:PAST + nt, :])
        self.flag_sx = True
        yield 8.0
        for kt in range(8):
            for g in range(2):
                self.tr(pt[:, g * 128:(g + 1) * 128], cstage[:, kt, g * 128:(g + 1) * 128], self.ident)
            self.tr(pt[:, 256:384], kistage[:, kt, :], self.ident)
            self.cp(kv["KT"][:, :, kt * 128:(kt + 1) * 128].rk(("sKT", kt)), pt[:, 0:256].rr("p (g t) -> p g t", g=2), "dve")
            self.cp(kv["KI"][:, kt * 128:(kt + 1) * 128].rk(("sKI", kt)), pt[:, 256:384], "dve")
            yield 3.0
        w = self.wget("sm")
        self.sh_sm(0, nt, w, 8, O["ki_s"], kv=kv)
        self.own_wi(w, d["xT"], nt, self.wi2[0])
        self.flag_sk = True
        yield 12.0
        self.sh_gk(0, nt, self.wget("gk"))
        yield 5.0
        for c in range(2):
            self.sh_gv(0, nt, self.wget(f"gv{c}"), c)
            yield 4.0
        self.sh_dkv(0, nt, self.wget("dkv"), 8, O["k_s"], O["v_s"], kv=kv)
        yield 7.0
        self.sh_state(0, nt)
        self.dma("sp", O["st_s"].rr("h k v -> k h v"), self.S)
        yield 6.0
        while not self.back_done:
            yield 0.0
        yield from self.g_front_gla(**self.sample_args())

    def g_sample_pre2(self, wait):
        while not self.flag_sx:
            yield 0.0
        yield from self.g_front_q(wait=lambda: self.flag_sk and wait(), **self.sample_args())


    def phase2(self):
        I, O = self.inp, self.out
        while self.we_chunks:
            c = self.we_chunks.pop(0)
            a_, b_ = c * self.CH, min(WE_TOT, (c + 1) * self.CH)
            self.dma("pool", self.we16[:, a_:b_].rk(("we16", c)), I["we"][:, a_:b_])
        self.prog.barrier()
        ar = self.ar
        ar.off = self.p2_mark
        A = ar.alloc
        NS = 17
        slots = [(s, 128) for s in range(16)] + [(16, DEC_T)]
        hacc = A([NS, 1024], F32)
        hT = A([8, NS * 128], BF16)
        hb = A([1024], BF16)
        wr = A([8, 20], BF16)
        self.dma("pool", wr, I["wr"].rr("p (k c) -> p k c", k=8))
        brt = A([20], F32)
        self.dma("sp", brt, Buf(I["br"].ap.partition_broadcast(128), None))
        ln1 = A([2, 1024], F32)
        self.dma("sp", ln1[:, 0, :], Buf(I["ln"].ap[0:1, :].partition_broadcast(128), None))
        self.dma("sp", ln1[:, 1, :], Buf(I["ln"].ap[1:2, :].partition_broadcast(128), None))
        ln2 = A([2, 1024], F32)
        self.dma("sp", ln2[:, 0, :], Buf(I["ln"].ap[2:3, :].partition_broadcast(128), None))
        self.dma("sp", ln2[:, 1, :], Buf(I["ln"].ap[3:4, :].partition_broadcast(128), None))
        sel = A([16 * 128], F32, parts=16)
        self.dma("sp", sel, I["sel"])
        gateT = A([NS * 128], F32, parts=16)
        sm = A([128], F32)
        NR = 3
        ering = [A([WE_PER], BF16, key=("ering", i)) for i in range(NR)]
        sgb = A([2, 512], BF16)
        gbs = A([512], F32)
        tb = A([2, 512], BF16)
        EG = 2
        hid = [A([2, 512], BF16) for _ in range(EG)]
        print("phase-2 SBUF bytes/partition:", ar.off)
        pt = self.ps(2, BF16)
        for s, nt in slots:
            hs = hacc[:, s, :]
            self.dma("sp", hs[0:nt, :], self.hscr[s * 128:s * 128 + nt, :].rk(("hscr", s)))
            self.tt(hs[0:nt, :], hs[0:nt, :], ln1[0:nt, 0, :], ALU.mult)
            self.tt(hs[0:nt, :], hs[0:nt, :], ln1[0:nt, 1, :], ALU.add)
            self.cp(hb[0:nt, :], hs[0:nt, :], "act")
            for k in range(8):
                self.tr(pt[:, k * nt:(k + 1) * nt], hb[0:nt, k * 128:(k + 1) * 128], self.ident[0:nt, 0:nt])
            self.cp(hT[:, :, s * 128:s * 128 + nt], pt[:, 0:8 * nt].rr("p (k t) -> p k t", k=8), "dve")
            pz = self.ps(0)
            for k in range(8):
                self.mm(pz[0:nt, 0:20], hT[:, k, s * 128:s * 128 + nt], wr[:, k, :], start=(k == 0), stop=(k == 7))
            lg = sm[:, 0:20]
            self.tt(lg[0:nt, :], pz[0:nt, 0:20], brt[0:nt, :], ALU.add)
            gmax, nge, sume, pg = sm[:, 20:21], sm[:, 21:22], sm[:, 22:23], sm[:, 23:24]
            goh = sm[:, 24:28]
            em = sm[:, 32:48]
            oh1 = sm[:, 48:64]
            oh2 = sm[:, 64:80]
            m1, m2, e2, w1, w2 = sm[:, 80:81], sm[:, 81:82], sm[:, 82:83], sm[:, 83:84], sm[:, 84:85]
            junk = sm[:, 88:92]
            gate = sm[:, 96:112]
            self.raw("dve", "tensor_reduce", [gmax], [lg], out=gmax[0:nt, :], in_=lg[0:nt, 0:4], axis=AX.X, op=ALU.max)
            self.ts(nge[0:nt, :], gmax[0:nt, :], -1.0, ALU.mult)
            self.act(junk[0:nt, :], lg[0:nt, 0:4], AF.Exp, bias=nge[0:nt, :], accum=sume[0:nt, :])
            self.raw("dve", "reciprocal", [pg], [sume], out=pg[0:nt, :], in_=sume[0:nt, :])
            self.ts(goh[0:nt, :], lg[0:nt, 0:4], gmax[0:nt, :], ALU.is_ge, 1.0, ALU.subtract)
            self.ts(goh[0:nt, :], goh[0:nt, :], 1e30, ALU.mult)
            self.tt(em[0:nt, :].rr("p (g e) -> p g e", g=4), lg[0:nt, 4:20].rr("p (g e) -> p g e", g=4),
                    goh[0:nt, :].uq(2).bt([nt, 4, 4]), ALU.add)
            self.raw("dve", "tensor_reduce", [m1], [em], out=m1[0:nt, :], in_=em[0:nt, :], axis=AX.X, op=ALU.max)
            self.ts(oh1[0:nt, :], em[0:nt, :], m1[0:nt, :], ALU.is_ge)
            self.stt(em[0:nt, :], oh1[0:nt, :], -1e30, em[0:nt, :], ALU.mult, ALU.add)
            self.raw("dve", "tensor_reduce", [m2], [em], out=m2[0:nt, :], in_=em[0:nt, :], axis=AX.X, op=ALU.max)
            self.ts(oh2[0:nt, :], em[0:nt, :], m2[0:nt, :], ALU.is_ge)
            self.ts(nge[0:nt, :], m1[0:nt, :], -1.0, ALU.mult)
            self.act(e2[0:nt, :], m2[0:nt, :], AF.Exp, bias=nge[0:nt, :])
            self.ts(w1[0:nt, :], e2[0:nt, :], 1.0, ALU.add)
            self.raw("dve", "reciprocal", [w1], [w1], out=w1[0:nt, :], in_=w1[0:nt, :])
            self.tt(w1[0:nt, :], w1[0:nt, :], pg[0:nt, :], ALU.mult)
            self.tt(w2[0:nt, :], w1[0:nt, :], e2[0:nt, :], ALU.mult)
            self.ts(gate[0:nt, :], oh1[0:nt, :], w1[0:nt, :], ALU.mult)
            self.stt(gate[0:nt, :], oh2[0:nt, :], w2[0:nt, :], gate[0:nt, :], ALU.mult, ALU.add)
            pg_ = self.ps(1)
            self.tr(pg_[0:16, 0:nt], gate[0:nt, :], self.identf[0:nt, 0:nt])
            self.cp(gateT[0:16, s * 128:s * 128 + nt], pg_[0:16, 0:nt], "act")
            self.ts(hs[0:nt, :], hs[0:nt, :], ALPHA, ALU.mult)
        batches = [(b * 512, 512, [(4 * b + i, i * 128, 128) for i in range(4)]) for b in range(4)]
        batches.append((2048, DEC_T, [(16, 0, DEC_T)]))
        issued = 0

        def eload(upto):
            nonlocal issued
            while issued < min(16, upto):
                e = issued
                o0 = e * WE_PER
                chs = MK(("we16", q) for q in range(o0 // self.CH, (o0 + WE_PER - 1) // self.CH + 1))
                self.dma("sp", ering[e % NR], self.we16[:, o0:o0 + WE_PER].rk(chs))
                issued += 1

        pd = 0
        for g0 in range(0, 16, EG):
            eload(g0 + NR)
            ws = []
            for e in range(g0, g0 + EG):
                r = ering[e % NR]
                ws.append((r[:, 0:4096].rr("p (k c) -> p k c", k=8), r[:, 4096:WE_PER].rr("p (f c) -> p f c", f=2)))
            for c0, N, bslots in batches:
                for ei in range(EG):
                    e = g0 + ei
                    wgu, wd = ws[ei]
                    for fc in range(2):
                        pG, pU = self.ps(fc), self.ps(2 + fc)
                        for k in range(8):
                            self.mm(pG[:, 0:N], wgu[:, k, fc * 128:(fc + 1) * 128], hT[:, k, c0:c0 + N],
                                    start=(k == 0), stop=(k == 7))
                        for k in range(8):
                            self.mm(pU[:, 0:N], wgu[:, k, 256 + fc * 128:256 + (fc + 1) * 128], hT[:, k, c0:c0 + N],
                                    start=(k == 0), stop=(k == 7))
                    pB = self.ps(4)
                    self.mm(pB[:, 0:N], sel[0:16, e * 128:(e + 1) * 128], gateT[0:16, c0:c0 + N])
                    for fc in range(2):
                        self.act(sgb[:, fc, 0:N], self.ps(fc)[:, 0:N], AF.Silu)
                    self.cp(gbs[:, 0:N], pB[:, 0:N], "act")
                    for fc in range(2):
                        self.tt(tb[:, fc, 0:N], self.ps(2 + fc)[:, 0:N], sgb[:, fc, 0:N], ALU.mult)
                    self.tt(hid[ei][:, :, 0:N], tb[:, :, 0:N], gbs[:, 0:N].uq(1).bt([128, 2, N]), ALU.mult, eng="pool")
                for s, off, nt in bslots:
                    for c in range(2):
                        pD = self.ps(5 + pd % 3)
                        pd += 1
                        n_acc = 2 * EG
                        a = 0
                        for ei in range(EG):
                            for fc in range(2):
                                self.mm(pD[0:nt, :], hid[ei][:, fc, off:off + nt], ws[ei][1][:, fc, c * 512:(c + 1) * 512],
                                        start=(a == 0), stop=(a == n_acc - 1))
                                a += 1
                        self.tt(hacc[0:nt, s, c * 512:(c + 1) * 512], pD[0:nt, :], hacc[0:nt, s, c * 512:(c + 1) * 512], ALU.add)
        for s, nt in slots:
            hs = hacc[:, s, :]
            st, mv, rs = sm[:, 0:12], sm[:, 12:14], sm[:, 14:15]
            self.layernorm_stats(hs, nt, 2, 512, mv, st, rs)
            self.ts(hs[0:nt, :], hs[0:nt, :], mv[0:nt, 0:1], ALU.subtract, rs[0:nt, 0:1], ALU.mult)
            self.tt(hs[0:nt, :], hs[0:nt, :], ln2[0:nt, 0, :], ALU.mult)
            self.tt(hs[0:nt, :], hs[0:nt, :], ln2[0:nt, 1, :], ALU.add)
            dst = O["y_own"][s * 128:(s + 1) * 128, :] if s < 16 else O["y_smp"]
            self.dma("sp", dst, hs[0:nt, :])


def _rope_table(pos):
    pos = np.asarray(pos, np.float32)
    out = np.zeros((len(pos), 192), np.float32)
    for half, c0 in ((64, 0), (32, 128)):
        inv = (10000.0 ** (-np.arange(half, dtype=np.float32) / half)).astype(np.float32)
        ang = pos[:, None] * inv[None, :]
        out[:, c0:c0 + half] = np.cos(ang)
        out[:, c0 + half:c0 + 2 * half] = np.sin(ang)
    return out


def _consts(p):
    c = np.zeros((128, C_TOT), np.float32)
    r = np.arange(128)
    c[:, _C["ident"][0]:_C["ident"][1]] = np.eye(128, dtype=np.float32)
    tri = (r[:, None] <= r[None, :]).astype(np.float32)
    c[:, _C["uneg"][0]:_C["uneg"][1]] = tri * (-1.0 / 16.0)
    c[:, _C["cm"][0]:_C["cm"][1]] = tri
    c[:, _C["pow2"][0]:_C["pow2"][1]] = (0.5 ** (np.arange(32) + 1))[None, :]
    c[:, _C["pm"][0]] = 1.0 - p
    c[:, _C["pm"][0] + 1] = float(p)
    rr = r + 128 * p
    lim = (rr // 64 + 1) * 64
    s = np.arange(256)
    c[:, _C["admb"][0]:_C["admb"][1]] = np.where(s[None, :] < lim[:, None], 0.0, -1e30)
    return c


def _pack_blocks(w_in, wbg, wbd, wo):
    src = {"in": w_in, "bg": wbg, "bd": wbd, "wo": wo}
    out = np.empty((128, WA_TOT), np.float32)
    for n, parts in _BLK:
        m = np.concatenate([src[s][:, a:b] for s, a, b in parts], axis=1)
        c = m.shape[1]
        out[:, _BLK_OFF[n]:_BLK_OFF[n] + 8 * c] = m.reshape(8, 128, c).transpose(1, 0, 2).reshape(128, 8 * c)
    return out


def _pack_experts(wg, wu, wd):
    out = np.empty((128, WE_TOT), np.float32)
    for e in range(16):
        gu = np.concatenate([wg[e], wu[e]], axis=1)
        o = e * WE_PER
        out[:, o:o + 4096] = gu.reshape(8, 128, 512).transpose(1, 0, 2).reshape(128, 4096)
        out[:, o + 4096:o + WE_PER] = wd[e].reshape(2, 128, 1024).transpose(1, 0, 2).reshape(128, 2048)
    return out


STAGE = 4
_CACHE = {}


def kernel(x_prompt, x_sample, state_gla, cache_k, cache_v, cache_k_idx, w_in, w_gla_gate_up, b_gla_gate,
           gla_norm_g, gla_norm_b, w_branch_gla, w_branch_dsa, w_out, ln1_g, ln1_b, w_router_group,
           b_router_group, w_router_expert, b_router_expert, w_expert_gate, w_expert_up, w_expert_down,
           ln2_g, ln2_b):
    f = lambda a: np.ascontiguousarray(np.asarray(a, dtype=np.float32))
    x_prompt, x_sample = f(x_prompt), f(x_sample)
    wa = _pack_blocks(f(w_in)[0], f(w_branch_gla)[0], f(w_branch_dsa)[0], f(w_out)[0])
    we = _pack_experts(f(w_expert_gate)[0], f(w_expert_up)[0], f(w_expert_down)[0])
    wup = np.concatenate([f(w_gla_gate_up)[0], f(b_gla_gate)], axis=0)
    gn = np.concatenate([f(gla_norm_g), f(gla_norm_b)], axis=0)
    ln = np.concatenate([f(ln1_g), f(ln1_b), f(ln2_g), f(ln2_b)], axis=0)
    wrc = np.concatenate([f(w_router_group)[0]] + [f(w_router_expert)[0, g] for g in range(4)], axis=1)
    wr = wrc.reshape(8, 128, 20).transpose(1, 0, 2).reshape(128, 160)
    br = np.concatenate([f(b_router_group)[0], f(b_router_expert)[0].reshape(-1)])[None, :]
    sel = np.zeros((16, 16, 128), np.float32)
    for e in range(16):
        sel[e, e, :] = 1.0
    sel = sel.reshape(16, 2048)
    rope_all = _rope_table(np.arange(SEQ))
    in_maps = []
    for c in range(8):
        b, p = c // 2, c % 2
        xs = x_prompt[b]
        own_rows = (np.arange(NPAIR)[:, None] * 256 + p * 128 + np.arange(128)[None, :]).reshape(-1)
        in_maps.append({
            "x_seq": xs, "x_own": np.ascontiguousarray(xs[own_rows]), "x_smp": x_sample[c],
            "s0": f(state_gla)[0, c], "ck": f(cache_k)[0, c].reshape(PAST, 256),
            "cv": f(cache_v)[0, c].reshape(PAST, 256), "cki": f(cache_k_idx)[0, c],
            "wa": wa, "we": we, "wup": wup, "gn": gn, "ln": ln, "wr": wr, "br": br, "sel": sel,
            "cst": _consts(p), "rope_seq": rope_all, "rope_own": np.ascontiguousarray(rope_all[own_rows]),
        })
    if "nc" not in _CACHE:
        planner = K(STAGE)
        planner.build()
        _CACHE["nc"] = K(STAGE, wseq=planner.wseq).build()
    res = run_bass_kernel_spmd(_CACHE["nc"], in_maps, core_ids=list(range(8)))
    R = res.results
    y_p = np.zeros((4, SEQ, D), np.float32)
    for c in range(8):
        b, p = c // 2, c % 2
        y_p[b].reshape(NPAIR, 2, 128, D)[:, p] = R[c]["y_own"].reshape(NPAIR, 128, D)
    y_s = np.stack([R[c]["y_smp"] for c in range(8)])
    st_p = np.stack([R[2 * b]["st_p"] for b in range(4)])[None]
    k_p = np.stack([R[2 * b]["k_p"].reshape(SEQ, 2, 128) for b in range(4)])[None]
    v_p = np.stack([R[2 * b]["v_p"].reshape(SEQ, 2, 128) for b in range(4)])[None]
    ki_p = np.stack([R[2 * b]["ki_p"] for b in range(4)])[None]
    st_s = np.stack([R[c]["st_s"] for c in range(8)])[None]
    k_s = np.stack([R[c]["k_s"].reshape(DEC_T, 2, 128) for c in range(8)])[None]
    v_s = np.stack([R[c]["v_s"].reshape(DEC_T, 2, 128) for c in range(8)])[None]
    ki_s = np.stack([R[c]["ki_s"] for c in range(8)])[None]
    return (y_p, y_s, st_p, k_p, v_p, ki_p, st_s, k_s, v_s, ki_s)
```
